# Optimizing a Trainium2 kernel written in Bass

```python
import math
import jax, jax.numpy as jnp
from jax import lax
import numpy as np

D_MODEL = 1024
BATCH = 4
SEQ = 4096
DEPTH = 2

GRID_W = 64
CTX_LEN = 256
N_EVEN = (DEPTH + 1) // 2
N_ODD = DEPTH // 2

LRU_WIDTH = D_MODEL // 2
LRU_BLOCKS = 8
LRU_BLOCK = LRU_WIDTH // LRU_BLOCKS
CONV_W = 4
LRU_C = 8.0

MLA_HEADS = 8
MLA_NOPE = 64
MLA_ROPE = 32
MLA_QK = MLA_NOPE + MLA_ROPE
MLA_V = 64
Q_LORA = D_MODEL // 4
KV_LORA = D_MODEL // 8

EVEN_SPLITS = (LRU_WIDTH, 2 * LRU_WIDTH, 2 * LRU_WIDTH + Q_LORA, 2 * LRU_WIDTH + Q_LORA + KV_LORA)
EVEN_IN = 2 * LRU_WIDTH + Q_LORA + KV_LORA + MLA_ROPE
EVEN_MIX = LRU_WIDTH + MLA_HEADS * MLA_V

GQA_HEADS = 16
GQA_KV_HEADS = 4
GQA_DIM = 64
WINDOW = 128
ODD_IN = (GQA_HEADS + 2 * GQA_KV_HEADS) * GQA_DIM
ODD_MIX = GQA_HEADS * GQA_DIM

Q_BLOCK = 128
ROPE_THETA = 10000.0
NEG_INF = -1e30
EPS = 1e-6

N_EXPERTS = 32
TOP_K = 4
D_FF = D_MODEL
SWIGLU_LIMIT = 7.0
SWIGLU_ALPHA = 1.702
MOE_BLOCK = 128

kernel_name = 'hybrid_rglru_mla_swa_moe_diffusion_block'


def rms_norm(x, g):
    xf = x.astype(jnp.float32)
    y = xf * lax.rsqrt(jnp.mean(xf * xf, axis=-1, keepdims=True) + EPS)
    return (y * g.astype(jnp.float32)).astype(x.dtype)


def axial_rope(n_rows, rot_dim):
    row = jnp.repeat(jnp.arange(n_rows, dtype=jnp.float32), GRID_W)
    col = jnp.tile(jnp.arange(GRID_W, dtype=jnp.float32), n_rows)
    n = rot_dim // 4
    freqs = ROPE_THETA ** (-jnp.arange(n, dtype=jnp.float32) / n)
    ang = jnp.concatenate([row[:, None] * freqs, col[:, None] * freqs], axis=-1)
    return jnp.cos(ang), jnp.sin(ang)


def apply_rope(x, cos, sin):
    xf = x.astype(jnp.float32)
    x1, x2 = jnp.split(xf, 2, axis=-1)
    cs, sn = cos[None, :, None, :], sin[None, :, None, :]
    return jnp.concatenate([x1 * cs - x2 * sn, x2 * cs + x1 * sn], axis=-1).astype(x.dtype)


def ctx_attention(q, k, v, scale, sink=None):
    B, L, H, d = q.shape
    KH = k.shape[2]
    G = H // KH
    qg = q.reshape(B, L, KH, G, d)
    s = jnp.einsum('bqhgd,bkhd->bhgqk', qg, k, preferred_element_type=jnp.float32) * scale
    if sink is not None:
        sink_logit = jnp.broadcast_to(sink.astype(jnp.float32).reshape(1, KH, G, 1, 1), (B, KH, G, L, 1))
        s = jnp.concatenate([s, sink_logit], axis=-1)
    p = jax.nn.softmax(s, axis=-1)[..., :L].astype(v.dtype)
    o = jnp.einsum('bhgqk,bkhd->bqhgd', p, v)
    return o.reshape(B, L, H, -1)


def dense_latent_attention(q, k, v, scale):
    B, S, H, dq = q.shape
    nb = S // Q_BLOCK
    q_blocks = q.reshape(B, nb, Q_BLOCK, H, dq).swapaxes(0, 1)

    def one_block(q_blk):
        s = jnp.einsum('bqhd,bkhd->bhqk', q_blk, k, preferred_element_type=jnp.float32) * scale
        p = jax.nn.softmax(s, axis=-1).astype(v.dtype)
        return jnp.einsum('bhqk,bkhd->bqhd', p, v)

    o = lax.map(one_block, q_blocks)
    return o.swapaxes(0, 1).reshape(B, S, H, -1)


def window_attention(q, k, v, k_ctx, v_ctx, sink, scale):
    B, S, H, d = q.shape
    KH = k.shape[2]
    G = H // KH
    L = k_ctx.shape[1]
    W3 = 3 * Q_BLOCK
    nb = S // Q_BLOCK
    pad = ((0, 0), (Q_BLOCK, Q_BLOCK), (0, 0), (0, 0))
    k_pad = jnp.pad(k, pad)
    v_pad = jnp.pad(v, pad)
    q_blocks = q.reshape(B, nb, Q_BLOCK, KH, G, d).swapaxes(0, 1)
    sink_logit = jnp.broadcast_to(sink.astype(jnp.float32).reshape(1, KH, G, 1, 1), (B, KH, G, Q_BLOCK, 1))

    def one_block(args):
        i, q_blk = args
        k_win = lax.dynamic_slice_in_dim(k_pad, i * Q_BLOCK, W3, axis=1)
        v_win = lax.dynamic_slice_in_dim(v_pad, i * Q_BLOCK, W3, axis=1)
        q_pos = i * Q_BLOCK + jnp.arange(Q_BLOCK)
        k_pos = (i - 1) * Q_BLOCK + jnp.arange(W3)
        valid = (jnp.abs(q_pos[:, None] - k_pos[None, :]) <= WINDOW) & (k_pos >= 0) & (k_pos < S)
        s_win = jnp.einsum('bqhgd,bkhd->bhgqk', q_blk, k_win, preferred_element_type=jnp.float32) * scale
        s_win = jnp.where(valid, s_win, NEG_INF)
        s_ctx = jnp.einsum('bqhgd,bkhd->bhgqk', q_blk, k_ctx, preferred_element_type=jnp.float32) * scale
        p = jax.nn.softmax(jnp.concatenate([s_win, s_ctx, sink_logit], axis=-1), axis=-1)
        p_win = p[..., :W3].astype(v.dtype)
        p_ctx = p[..., W3:W3 + L].astype(v.dtype)
        return (jnp.einsum('bhgqk,bkhd->bqhgd', p_win, v_win)
                + jnp.einsum('bhgqk,bkhd->bqhgd', p_ctx, v_ctx))

    o = lax.map(one_block, (jnp.arange(nb), q_blocks))
    return o.swapaxes(0, 1).reshape(B, S, H, d)


def _linear_recurrence_combine(e1, e2):
    a1, b1 = e1
    a2, b2 = e2
    return a1 * a2, a2 * b1 + b2


def rglru_scan(u, conv_w, conv_b, w_r, b_r, w_i, b_i, lam, h0):
    B, T, C = u.shape
    xc = lax.conv_general_dilated(
        u, conv_w.astype(u.dtype)[:, None, :], window_strides=(1,), padding=[(CONV_W - 1, 0)],
        dimension_numbers=('NWC', 'WIO', 'NWC'), feature_group_count=C)
    xf = (xc + conv_b.astype(u.dtype)).astype(jnp.float32)
    xb = xf.reshape(B, T, LRU_BLOCKS, LRU_BLOCK)
    r = jax.nn.sigmoid(jnp.einsum('btnc,ncd->btnd', xb, w_r.astype(jnp.float32)).reshape(B, T, C)
                       + b_r.astype(jnp.float32))
    gi = jax.nn.sigmoid(jnp.einsum('btnc,ncd->btnd', xb, w_i.astype(jnp.float32)).reshape(B, T, C)
                        + b_i.astype(jnp.float32))
    log_a = -LRU_C * r * jax.nn.softplus(-lam.astype(jnp.float32))
    a = jnp.exp(log_a)
    b = jnp.sqrt(-jnp.expm1(2.0 * log_a)) * (gi * xf)
    b = b.at[:, 0].add(a[:, 0] * h0)
    _, h = lax.associative_scan(_linear_recurrence_combine, (a, b), axis=1)
    return h, h[:, -1]


def even_mixer(h_lat, h_ctx, w_in, conv_w, conv_b, w_r, b_r, w_i, b_i, lam,
               q_a_norm, w_q_b, kv_a_norm, w_kv_b, nope_norm, rope_norm, w_out, rope_cs, need_ctx):
    B, S, _ = h_lat.shape
    L = h_ctx.shape[1]
    xa_l, ga_l, qa_l, kva_l, kr_l = jnp.split(h_lat @ w_in, EVEN_SPLITS, axis=-1)
    xa_c, ga_c, qa_c, kva_c, kr_c = jnp.split(h_ctx @ w_in, EVEN_SPLITS, axis=-1)

    rec_l, rec_c = [], []
    for d in range(2):
        flip = (lambda t: jnp.flip(t, axis=1)) if d == 1 else (lambda t: t)
        h0 = jnp.zeros((B, LRU_WIDTH), jnp.float32)
        hc, hc_last = rglru_scan(flip(xa_c), conv_w[d], conv_b[d], w_r[d], b_r[d], w_i[d], b_i[d], lam[d], h0)
        hl, _ = rglru_scan(flip(xa_l), conv_w[d], conv_b[d], w_r[d], b_r[d], w_i[d], b_i[d], lam[d], hc_last)
        rec_l.append(flip(hl))
        rec_c.append(flip(hc))
    ya_l = ((rec_l[0] + rec_l[1]) * jax.nn.gelu(ga_l.astype(jnp.float32))).astype(h_lat.dtype)

    def mla_qkv(qa, kva, kr, cs):
        Bq, T, _ = qa.shape
        q = (rms_norm(qa, q_a_norm) @ w_q_b).reshape(Bq, T, MLA_HEADS, MLA_QK)
        kv = (rms_norm(kva, kv_a_norm) @ w_kv_b).reshape(Bq, T, MLA_HEADS, MLA_NOPE + MLA_V)
        q_nope = rms_norm(q[..., :MLA_NOPE], nope_norm[0])
        q_rope = rms_norm(q[..., MLA_NOPE:], rope_norm[0])
        k_nope = rms_norm(kv[..., :MLA_NOPE], nope_norm[1])
        v = kv[..., MLA_NOPE:]
        k_rope = rms_norm(kr[:, :, None, :], rope_norm[1])
        if cs is not None:
            q_rope = apply_rope(q_rope, *cs)
            k_rope = apply_rope(k_rope, *cs)
        k_rope = jnp.broadcast_to(k_rope, (Bq, T, MLA_HEADS, MLA_ROPE))
        return (jnp.concatenate([q_nope, q_rope], axis=-1),
                jnp.concatenate([k_nope, k_rope], axis=-1), v)

    scale = MLA_QK ** -0.5
    q_c, k_c, v_c = mla_qkv(qa_c, kva_c, kr_c, None)
    q_l, k_l, v_l = mla_qkv(qa_l, kva_l, kr_l, rope_cs)
    yb_l = dense_latent_attention(q_l, jnp.concatenate([k_l, k_c], axis=1),
                                  jnp.concatenate([v_l, v_c], axis=1), scale)
    out_l = jnp.concatenate([ya_l, yb_l.reshape(B, S, -1)], axis=-1) @ w_out
    out_c = None
    if need_ctx:
        ya_c = ((rec_c[0] + rec_c[1]) * jax.nn.gelu(ga_c.astype(jnp.float32))).astype(h_ctx.dtype)
        yb_c = ctx_attention(q_c, k_c, v_c, scale)
        out_c = jnp.concatenate([ya_c, yb_c.reshape(B, L, -1)], axis=-1) @ w_out
    return out_l, out_c


def odd_mixer(h_lat, h_ctx, w_qkv, qk_norm, sink, w_out, rope_cs, need_ctx):
    B, S, _ = h_lat.shape
    L = h_ctx.shape[1]
    nq = GQA_HEADS * GQA_DIM
    nk = GQA_KV_HEADS * GQA_DIM

    def qkv(h, cs):
        Bq, T, _ = h.shape
        z = h @ w_qkv
        q = rms_norm(z[..., :nq].reshape(Bq, T, GQA_HEADS, GQA_DIM), qk_norm[0])
        k = rms_norm(z[..., nq:nq + nk].reshape(Bq, T, GQA_KV_HEADS, GQA_DIM), qk_norm[1])
        v = z[..., nq + nk:].reshape(Bq, T, GQA_KV_HEADS, GQA_DIM)
        if cs is not None:
            q = apply_rope(q, *cs)
            k = apply_rope(k, *cs)
        return q, k, v

    scale = GQA_DIM ** -0.5
    q_c, k_c, v_c = qkv(h_ctx, None)
    q_l, k_l, v_l = qkv(h_lat, rope_cs)
    o_l = window_attention(q_l, k_l, v_l, k_c, v_c, sink, scale)
    out_l = o_l.reshape(B, S, -1) @ w_out
    out_c = None
    if need_ctx:
        out_c = ctx_attention(q_c, k_c, v_c, scale, sink).reshape(B, L, -1) @ w_out
    return out_l, out_c


def moe_ffn(t, w_router, b_router, w_gu, b_gu, w_dn, b_dn):
    N, D = t.shape
    logits = jnp.dot(t, w_router, preferred_element_type=jnp.float32) + b_router.astype(jnp.float32)
    top_logit, top_e = lax.top_k(logits, TOP_K)
    gate = jax.nn.softmax(top_logit, axis=-1)
    n_assign = N * TOP_K
    flat_e = top_e.reshape(-1)
    order = jnp.argsort(flat_e)
    e_sorted = flat_e[order]
    tok_sorted = (order // TOP_K).astype(jnp.int32)
    gate_sorted = gate.reshape(-1)[order]
    counts = jnp.bincount(flat_e, length=N_EXPERTS)
    padded = (counts + MOE_BLOCK - 1) // MOE_BLOCK * MOE_BLOCK
    start = jnp.cumsum(counts) - counts
    padded_end = jnp.cumsum(padded)
    padded_start = padded_end - padded
    dest = padded_start[e_sorted] + jnp.arange(n_assign) - start[e_sorted]
    n_blocks = -(-n_assign // MOE_BLOCK) + N_EXPERTS
    rows = n_blocks * MOE_BLOCK
    row_tok = jnp.full((rows,), N, jnp.int32).at[dest].set(tok_sorted)
    row_gate = jnp.zeros((rows,), jnp.float32).at[dest].set(gate_sorted)
    block_e = jnp.minimum(jnp.searchsorted(padded_end, jnp.arange(n_blocks) * MOE_BLOCK, side='right'),
                          N_EXPERTS - 1)
    t_pad = jnp.concatenate([t, jnp.zeros((1, D), t.dtype)], axis=0)
    xb = t_pad[row_tok].reshape(n_blocks, MOE_BLOCK, D)

    def expert_block(args):
        x_blk, e = args
        hgu = jnp.dot(x_blk, w_gu[e], preferred_element_type=jnp.float32) + b_gu[e].astype(jnp.float32)
        hg, hu = jnp.split(hgu, 2, axis=-1)
        hg = jnp.minimum(hg, SWIGLU_LIMIT)
        hu = jnp.clip(hu, -SWIGLU_LIMIT, SWIGLU_LIMIT)
        act = hg * jax.nn.sigmoid(SWIGLU_ALPHA * hg) * (hu + 1.0)
        return jnp.dot(act.astype(t.dtype), w_dn[e], preferred_element_type=jnp.float32) + b_dn[e].astype(jnp.float32)

    yb = lax.map(expert_block, (xb, block_e))
    y = yb.reshape(rows, D) * row_gate[:, None]
    out = jnp.zeros((N + 1, D), jnp.float32).at[row_tok].add(y)[:N]
    return out.astype(t.dtype)


def setup_inputs(seed: int = 0) -> dict:
    key = jax.random.key(seed)
    keys = iter(jax.random.split(key, 48))
    f32 = jnp.float32
    D = D_MODEL

    def nrm(shape, scale):
        return jax.random.normal(next(keys), shape, f32) * scale

    def gain(shape):
        return 1.0 + 0.1 * jax.random.normal(next(keys), shape, f32)

    x = nrm((BATCH, SEQ, D), 1.0)
    c = nrm((BATCH, D), 1.0)
    ctx = nrm((BATCH, CTX_LEN, D), 1.0)
    c_ctx = nrm((D,), 1.0)
    w_mod = nrm((DEPTH, D, 6 * D), 0.5 / math.sqrt(D))
    b_mod = nrm((DEPTH, 6 * D), 0.02)
    norm_mix = gain((DEPTH, D))
    norm_ffn = gain((DEPTH, D))
    w_in_even = nrm((N_EVEN, D, EVEN_IN), D ** -0.5)
    lru_conv_w = nrm((N_EVEN, 2, CONV_W, LRU_WIDTH), 0.5)
    lru_conv_b = nrm((N_EVEN, 2, LRU_WIDTH), 0.02)
    lru_w_r = nrm((N_EVEN, 2, LRU_BLOCKS, LRU_BLOCK, LRU_BLOCK), LRU_BLOCK ** -0.5)
    lru_b_r = nrm((N_EVEN, 2, LRU_WIDTH), 0.02)
    lru_w_i = nrm((N_EVEN, 2, LRU_BLOCKS, LRU_BLOCK, LRU_BLOCK), LRU_BLOCK ** -0.5)
    lru_b_i = nrm((N_EVEN, 2, LRU_WIDTH), 0.02)
    a0 = jax.random.uniform(next(keys), (N_EVEN, 2, LRU_WIDTH), f32, 0.9, 0.999)
    a_base = a0 ** (1.0 / LRU_C)
    lru_lambda = jnp.log(a_base) - jnp.log1p(-a_base)
    mla_q_a_norm = gain((N_EVEN, Q_LORA))
    mla_w_q_b = nrm((N_EVEN, Q_LORA, MLA_HEADS * MLA_QK), Q_LORA ** -0.5)
    mla_kv_a_norm = gain((N_EVEN, KV_LORA))
    mla_w_kv_b = nrm((N_EVEN, KV_LORA, MLA_HEADS * (MLA_NOPE + MLA_V)), KV_LORA ** -0.5)
    mla_nope_norm = gain((N_EVEN, 2, MLA_NOPE))
    mla_rope_norm = gain((N_EVEN, 2, MLA_ROPE))
    w_out_even = nrm((N_EVEN, EVEN_MIX, D), EVEN_MIX ** -0.5)
    w_qkv_odd = nrm((N_ODD, D, ODD_IN), D ** -0.5)
    gqa_qk_norm = gain((N_ODD, 2, GQA_DIM))
    gqa_sink = nrm((N_ODD, GQA_HEADS), 0.5)
    w_out_odd = nrm((N_ODD, ODD_MIX, D), ODD_MIX ** -0.5)
    w_router = nrm((DEPTH, D, N_EXPERTS), D ** -0.5)
    b_router = nrm((DEPTH, N_EXPERTS), 0.01)
    w_gate_up = nrm((DEPTH, N_EXPERTS, D, 2 * D_FF), D ** -0.5)
    b_gate_up = nrm((DEPTH, N_EXPERTS, 2 * D_FF), 0.02)
    w_down = nrm((DEPTH, N_EXPERTS, D_FF, D), D_FF ** -0.5)
    b_down = nrm((DEPTH, N_EXPERTS, D), 0.02)
    return {'x': x, 'c': c, 'ctx': ctx, 'c_ctx': c_ctx,
            'w_mod': w_mod, 'b_mod': b_mod, 'norm_mix': norm_mix, 'norm_ffn': norm_ffn,
            'w_in_even': w_in_even, 'lru_conv_w': lru_conv_w, 'lru_conv_b': lru_conv_b,
            'lru_w_r': lru_w_r, 'lru_b_r': lru_b_r, 'lru_w_i': lru_w_i, 'lru_b_i': lru_b_i,
            'lru_lambda': lru_lambda, 'mla_q_a_norm': mla_q_a_norm, 'mla_w_q_b': mla_w_q_b,
            'mla_kv_a_norm': mla_kv_a_norm, 'mla_w_kv_b': mla_w_kv_b, 'mla_nope_norm': mla_nope_norm,
            'mla_rope_norm': mla_rope_norm, 'w_out_even': w_out_even,
            'w_qkv_odd': w_qkv_odd, 'gqa_qk_norm': gqa_qk_norm, 'gqa_sink': gqa_sink, 'w_out_odd': w_out_odd,
            'w_router': w_router, 'b_router': b_router, 'w_gate_up': w_gate_up, 'b_gate_up': b_gate_up,
            'w_down': w_down, 'b_down': b_down}


def reference(x, c, ctx, c_ctx, w_mod, b_mod, norm_mix, norm_ffn,
              w_in_even, lru_conv_w, lru_conv_b, lru_w_r, lru_b_r, lru_w_i, lru_b_i, lru_lambda,
              mla_q_a_norm, mla_w_q_b, mla_kv_a_norm, mla_w_kv_b, mla_nope_norm, mla_rope_norm, w_out_even,
              w_qkv_odd, gqa_qk_norm, gqa_sink, w_out_odd,
              w_router, b_router, w_gate_up, b_gate_up, w_down, b_down):
    B, S, D = x.shape
    L = ctx.shape[1]
    n_rows = S // GRID_W
    rope_mla = axial_rope(n_rows, MLA_ROPE)
    rope_gqa = axial_rope(n_rows, GQA_DIM)
    x_l, x_c = x, ctx
    for layer in range(DEPTH):
        last = layer == DEPTH - 1
        mod_l = (jax.nn.silu(c) @ w_mod[layer] + b_mod[layer])[:, None, :]
        mod_c = jax.nn.silu(c_ctx) @ w_mod[layer] + b_mod[layer]
        sh1_l, sc1_l, g1_l, sh2_l, sc2_l, g2_l = jnp.split(mod_l, 6, axis=-1)
        sh1_c, sc1_c, g1_c, sh2_c, sc2_c, g2_c = jnp.split(mod_c, 6, axis=-1)
        h_l = rms_norm(x_l, norm_mix[layer]) * (1.0 + sc1_l) + sh1_l
        h_c = rms_norm(x_c, norm_mix[layer]) * (1.0 + sc1_c) + sh1_c
        if layer % 2 == 0:
            e = layer // 2
            m_l, m_c = even_mixer(h_l, h_c, w_in_even[e], lru_conv_w[e], lru_conv_b[e], lru_w_r[e], lru_b_r[e],
                                  lru_w_i[e], lru_b_i[e], lru_lambda[e], mla_q_a_norm[e], mla_w_q_b[e],
                                  mla_kv_a_norm[e], mla_w_kv_b[e], mla_nope_norm[e], mla_rope_norm[e],
                                  w_out_even[e], rope_mla, not last)
        else:
            o = layer // 2
            m_l, m_c = odd_mixer(h_l, h_c, w_qkv_odd[o], gqa_qk_norm[o], gqa_sink[o], w_out_odd[o],
                                 rope_gqa, not last)
        x_l = x_l + g1_l * m_l
        f_l = rms_norm(x_l, norm_ffn[layer]) * (1.0 + sc2_l) + sh2_l
        moe_args = (w_router[layer], b_router[layer], w_gate_up[layer], b_gate_up[layer],
                    w_down[layer], b_down[layer])
        if last:
            y_l = moe_ffn(f_l.reshape(B * S, D), *moe_args).reshape(B, S, D)
        else:
            x_c = x_c + g1_c * m_c
            f_c = rms_norm(x_c, norm_ffn[layer]) * (1.0 + sc2_c) + sh2_c
            y = moe_ffn(jnp.concatenate([f_l.reshape(B * S, D), f_c.reshape(B * L, D)], axis=0), *moe_args)
            y_l = y[:B * S].reshape(B, S, D)
            x_c = x_c + g2_c * y[B * S:].reshape(B, L, D)
        x_l = x_l + g2_l * y_l
    return x_l
```

```python
import math
from contextlib import ExitStack

import numpy as np
import concourse.bass as bass
import concourse.mybir as mybir
from concourse.bass_utils import run_bass_kernel_spmd

F32 = mybir.dt.float32
BF16 = mybir.dt.bfloat16
AF = mybir.ActivationFunctionType
ALU = mybir.AluOpType
AX = mybir.AxisListType

D = 1024
NCTX = 256
SEQ = 4096
NALL = NCTX + SEQ
NOWN = NCTX + 2048 + 128
NT_ALL = NALL // 128
NT_OWN = NOWN // 128
NE = 32
EPS = 1e-6


class Op:
    __slots__ = ("eng", "fn", "is_dma", "deps", "needs_inc", "ticket", "sem", "semval", "idx")


class Prog:
    CENG = ("pe", "act", "dve", "pool")

    def __init__(self, nc, stack):
        self.nc = nc
        self.ops = []
        self.esem = {e: stack.enter_context(nc.semaphore("s_" + e)) for e in self.CENG}
        self.dpool = {}
        for q, n in (("sp", 16), ("act", 8), ("pool", 16)):
            self.dpool[q] = [stack.enter_context(nc.semaphore("d_%s%d" % (q, i))) for i in range(n)]
        self.dcount = {q: 0 for q in self.dpool}
        self.last_dma = {}
        self.lastw = {}
        self.readers = {}
        self.last_on_eng = {}
        self.pending_bar = {}
        self.pskeys = set()

    def add(self, eng, fn, reads=(), writes=(), dma=False):
        xs = [r for r in reads if r in self.pskeys and r not in writes]
        if xs:
            writes = list(writes) + xs
        op = Op()
        op.eng, op.fn, op.is_dma = eng, fn, dma
        op.needs_inc, op.ticket, op.sem, op.semval = False, 0, None, 0
        op.idx = len(self.ops)
        deps = {}

        def dep(o, kind):
            if o is None or o is op:
                return
            if deps.get(o) != "raw":
                deps[o] = kind

        for r in reads:
            dep(self.lastw.get(r), "raw")
        for w in writes:
            dep(self.lastw.get(w), "w")
            for o in self.readers.get(w, ()):
                dep(o, "w")
        for o in self.pending_bar.pop(eng, ()):
            dep(o, "w")
        if dma:
            q = eng
            j = self.dcount[q]
            K = len(self.dpool[q])
            k = j % K
            dep(self.last_dma.get((q, k)), "raw")
            op.sem = self.dpool[q][k]
            op.semval = 16 * (j // K + 1)
            self.last_dma[(q, k)] = op
            self.dcount[q] += 1
            op.needs_inc = True
        else:
            op.sem = self.esem.get(eng)
        for r in reads:
            self.readers.setdefault(r, []).append(op)
        for w in writes:
            self.lastw[w] = op
            self.readers[w] = []
        kept = []
        for o, kind in deps.items():
            if (not o.is_dma) and (not op.is_dma) and o.eng == eng:
                if eng == "pe":
                    continue
            if not o.is_dma:
                o.needs_inc = True
            kept.append(o)
        op.deps = kept
        self.ops.append(op)
        if not dma:
            self.last_on_eng[eng] = op
        return op

    def barrier(self):
        outstanding = list(self.last_on_eng.values()) + list(self.last_dma.values())
        for e in ("pe", "act", "dve", "pool", "sp"):
            self.pending_bar[e] = list(self.pending_bar.get(e, ())) + outstanding
        self.lastw = {}
        self.readers = {}

    def emit(self):
        nc = self.nc
        cnt = {e: 0 for e in self.CENG}
        for op in self.ops:
            if not op.is_dma and op.needs_inc:
                cnt[op.eng] += 1
                op.semval = cnt[op.eng]
        by_eng = {e: [] for e in ("pe", "act", "dve", "pool", "sp")}
        for op in self.ops:
            by_eng[op.eng].append(op)

        def runner(name):
            def body(eng):
                known = {}
                for op in by_eng[name]:
                    for o in op.deps:
                        if known.get(o.sem, 0) >= o.semval:
                            continue
                        eng.wait_ge(o.sem, o.semval)
                        known[o.sem] = o.semval
                    if op.fn is None:
                        continue
                    ins = op.fn(eng)
                    if op.needs_inc:
                        ins.then_inc(op.sem, 16 if op.is_dma else 1)
            return body

        with nc.Block() as block:
            block.tensor(runner("pe"))
            block.scalar(runner("act"))
            block.vector(runner("dve"))
            block.gpsimd(runner("pool"))
            block.sync(runner("sp"))
        self.n_ops = {e: len(v) for e, v in by_eng.items()}


class Ctx:
    def __init__(self, nc, P):
        self.nc = nc
        self.P = P
        self.uid = 0

    def name(self, base):
        self.uid += 1
        return "%s_%d" % (base, self.uid)

    def sb(self, stack, base, shape, dtype):
        n = self.name(base)
        return stack.enter_context(self.nc.sbuf_tensor(n, list(shape), dtype))

    def ps(self, stack, base, shape, dtype=F32):
        n = self.name(base)
        t = stack.enter_context(self.nc.psum_tensor(n, [128, 512], F32))
        return t

    def pskey(self, key):
        self.P.pskeys.add(key)
        return key


def tile_groups(ntiles, maxt=4):
    out = []
    t = 0
    while t < ntiles:
        n = min(maxt, ntiles - t)
        out.append((t, n))
        t += n
    return out


def phase_mod(C, io):
    nc, P = C.nc, C.P
    with ExitStack() as st:
        cT = C.sb(st, "cT", [128, 16], F32)
        sg = C.sb(st, "csg", [128, 16], F32)
        cs = C.sb(st, "cs", [128, 16], F32)
        ones2 = C.sb(st, "ones2", [1, 2], F32)
        brow = [C.sb(st, "brow", [1, 512], F32) for _ in range(2)]
        wm = [C.sb(st, "wm", [128, 8, 512], F32) for _ in range(2)]
        orow = [C.sb(st, "orow", [2, 512], F32) for _ in range(2)]
        pm = [C.ps(st, "pm", [2, 512]) for _ in range(2)]
        for s_ in range(2):
            C.pskey(("pm", s_))
        P.add("sp", lambda e: e.dma_start(out=cT[:], in_=io["cT"]), writes=["cT"], dma=True)
        P.add("pool", lambda e: e.memset(ones2[:], 1.0), writes=["ones2"])
        P.add("act", lambda e: e.activation(sg[:], cT[:], AF.Sigmoid), reads=["cT"], writes=["csg"])
        P.add("dve", lambda e: e.tensor_tensor(cs[:], cT[:], sg[:], ALU.mult), reads=["cT", "csg"], writes=["cs"])
        it = 0
        for layer in range(2):
            for g in range(12):
                s = it % 2
                it += 1
                src = io["w_mod"][layer].rearrange("(k p) f -> p k f", p=128)[:, :, g * 512:(g + 1) * 512]
                P.add("sp", lambda e, s=s, src=src: e.dma_start(out=wm[s][:], in_=src),
                      writes=[("wm", s)], dma=True)
                bsrc = io["b_mod"][layer:layer + 1, g * 512:(g + 1) * 512]
                P.add("sp", lambda e, s=s, bsrc=bsrc: e.dma_start(out=brow[s][:], in_=bsrc),
                      writes=[("brow", s)], dma=True)
                for k in range(8):
                    P.add("pe", lambda e, s=s, k=k: e.matmul(pm[s][0:2, :], cs[:, 2 * k:2 * k + 2], wm[s][:, k, :],
                                                             start=(k == 0), stop=False),
                          reads=["cs", ("wm", s)], writes=[("pm", s)])
                P.add("pe", lambda e, s=s: e.matmul(pm[s][0:2, :], ones2[:], brow[s][:], start=False, stop=True),
                      reads=["ones2", ("brow", s)], writes=[("pm", s)])
                P.add("dve", lambda e, s=s: e.tensor_copy(orow[s][:], pm[s][0:2, :]), reads=[("pm", s)],
                      writes=[("orow", s)])
                dst = io["modrow"][layer, :, g * 512:(g + 1) * 512]
                P.add("sp", lambda e, s=s, dst=dst: e.dma_start(out=dst, in_=orow[s][:]),
                      reads=[("orow", s)], writes=["modrow"], dma=True)
    P.barrier()


class NormBufs:
    def __init__(self, C, st, tag):
        self.tag = tag
        self.junk = C.sb(st, "junk", [128, 1024], F32)
        self.xn = [C.sb(st, "xn", [128, 1024], F32) for _ in range(2)]
        self.ss = [C.sb(st, "ss", [128, 1], F32) for _ in range(2)]
        self.rs = [C.sb(st, "rs", [128, 1], F32) for _ in range(2)]
        self.pt = [C.ps(st, "pt", [128, 4, 128]) for _ in range(2)]
        for hh in range(2):
            C.pskey((tag, "pt", hh))
        self.i = 0


def norm_tile(C, NB, ident, xsrc, xkey, gm, sh, vec_keys, dst_bf, dst_key, dst_f32=None, dst32_key=None):
    P = C.P
    s = NB.i % 2
    NB.i += 1
    t = NB.tag
    junk, xn, ss, rs = NB.junk, NB.xn[s], NB.ss[s], NB.rs[s]
    P.add("act", lambda e: e.activation(junk[:], xsrc, AF.Square, accum_out=ss[:]),
          reads=[xkey], writes=[(t, "junk"), (t, "ss", s)])
    P.add("act", lambda e: e.activation(rs[:], ss[:], AF.Sqrt, bias=EPS, scale=1.0 / D),
          reads=[(t, "ss", s)], writes=[(t, "rs", s)])
    P.add("dve", lambda e: e.reciprocal(rs[:], rs[:]), reads=[(t, "rs", s)], writes=[(t, "rs", s)])
    P.add("dve", lambda e: e.tensor_scalar(xn[:], xsrc, rs[:, 0:1], None, ALU.mult),
          reads=[xkey, (t, "rs", s)], writes=[(t, "xn", s)])
    for hh in range(2):
        pt = NB.pt[hh]
        for kk in range(4):
            k = hh * 4 + kk
            P.add("pe", lambda e, k=k, kk=kk, pt=pt: e.transpose(pt[:, kk * 128:(kk + 1) * 128], xn[:, k * 128:(k + 1) * 128], ident[:]),
                  reads=[(t, "xn", s), "ident"], writes=[(t, "pt", hh)])
        for kk in range(4):
            k = hh * 4 + kk
            if dst_f32 is not None:
                P.add("dve", lambda e, k=k, kk=kk, pt=pt: e.tensor_scalar(dst_f32(k), pt[:, kk * 128:(kk + 1) * 128], gm[:, k:k + 1],
                                                                       sh[:, k:k + 1], ALU.mult, ALU.add),
                      reads=[(t, "pt", hh)] + list(vec_keys), writes=[dst32_key])
                P.add("act", lambda e, k=k: e.activation(dst_bf(k), dst_f32(k), AF.Copy),
                      reads=[dst32_key], writes=[dst_key])
            else:
                P.add("act", lambda e, k=k, kk=kk, pt=pt: e.activation(dst_bf(k), pt[:, kk * 128:(kk + 1) * 128], AF.Identity,
                                                                    bias=sh[:, k:k + 1], scale=gm[:, k:k + 1]),
                      reads=[(t, "pt", hh)] + list(vec_keys), writes=[dst_key])


def load_modcols(C, st, io, layer, which):
    P = C.P
    t = C.sb(st, "mc", [128, 2, 8], F32)
    key = C.name("mck")
    for r in range(2):
        src = io["modrow"][layer, r, which * 1024:(which + 1) * 1024].rearrange("(k p) -> p k", p=128)
        P.add("sp", lambda e, r=r, src=src: e.dma_start(out=t[:, r, :], in_=src, allow_slow_non_contiguous=True),
              reads=["modrow"], writes=[key], dma=True)
    return t, key


def phase_moe(C, io, layer, tiles, ctx_tiles, XL, out_ap=None, out_tiles=None):
    nc, P = C.nc, C.P
    NTL = len(tiles)
    NTOK = NTL * 128
    groups = tile_groups(NTL, 4)
    with ExitStack() as st:
        XS = C.sb(st, "XS", [128, NTL, 1024], F32)
        fT = C.sb(st, "fT", [128, 8, NTOK], BF16)
        gates = C.sb(st, "gates", [128, NTL, NE], F32)
        ident = C.sb(st, "ident", [128, 128], F32)
        P.add("sp", lambda e: e.dma_start(out=ident[:], in_=io["ident"]), writes=["ident"], dma=True)
        with ExitStack() as st2:
            NB = NormBufs(C, st2, "moenb")
            sc2, k1 = load_modcols(C, st2, io, layer, 4)
            sh2, k2 = load_modcols(C, st2, io, layer, 3)
            nf = C.sb(st2, "nf", [128, 8], F32)
            gm = C.sb(st2, "gm", [128, 2, 8], F32)
            P.add("sp", lambda e: e.dma_start(out=nf[:], in_=io["normT"][:, (2 * layer + 1) * 8:(2 * layer + 2) * 8]),
                  writes=["nf"], dma=True)
            for r in range(2):
                if C.lvl == 0:
                    continue
                P.add("dve", lambda e, r=r: e.scalar_tensor_tensor(gm[:, r, :], sc2[:, r, :], 1.0, nf[:], ALU.add, ALU.mult),
                      reads=[k1, "nf"], writes=["gmv"])
            wr = C.sb(st2, "wr", [128, 8, NE], F32)
            P.add("sp", lambda e: e.dma_start(out=wr[:], in_=io["w_router"][layer].rearrange("(k p) n -> p k n", p=128)),
                  writes=["wr"], dma=True)
            brb = C.sb(st2, "brb", [128, NE], F32)
            P.add("sp", lambda e: e.dma_start(out=brb[:], in_=io["b_router"][layer:layer + 1, :].partition_broadcast(128)),
                  writes=["brb"], dma=True)
            f32t = [C.sb(st2, "f32t", [128, 8, 128], F32) for _ in range(2)]
            pl = [C.ps(st2, "pl", [128, NE]) for _ in range(2)]
            for s_ in range(2):
                C.pskey(("pl", s_))
            lg = [C.sb(st2, "lg", [128, NE], F32) for _ in range(2)]
            m8 = [C.sb(st2, "m8", [128, 8], F32) for _ in range(2)]
            ex = [C.sb(st2, "ex", [128, NE], F32) for _ in range(2)]
            mk = [C.sb(st2, "mk", [128, NE], F32) for _ in range(2)]
            sm = [C.sb(st2, "sm", [128, 1], F32) for _ in range(2)]
            nm = [C.sb(st2, "nm", [128, 1], F32) for _ in range(2)]
            bdall = C.sb(st2, "bdall", [NE, 1024], F32)
            nea_ = io["nea"]
            if C.ne == NE:
                P.add("sp", lambda e: e.dma_start(out=bdall[:], in_=io["b_down"][layer * nea_:layer * nea_ + NE, :]), writes=["bdall"], dma=True)
            g2p = C.sb(st2, "g2p", [128, 2, 1024], F32)
            for r in range(2):
                src = io["modrow"][layer, r:r + 1, 5 * 1024:6 * 1024].partition_broadcast(128)
                P.add("sp", lambda e, r=r, src=src: e.dma_start(out=g2p[:, r, :], in_=src), reads=["modrow"], writes=["g2p"], dma=True)
            gT = [C.sb(st2, "gT", [NE, 128], F32) for _ in range(2)]
            btmp = [C.sb(st2, "btmp", [128, 512], F32) for _ in range(2)]
            pbias = [C.ps(st2, "pbias", [128, 512]) for _ in range(2)]
            for s_ in range(2):
                C.pskey(("pbias", s_))
            bi_ = 0
            for i, tix in enumerate(tiles):
                s = i % 2
                r = 1 if tix in ctx_tiles else 0
                P.add("sp", lambda e, i=i, tix=tix: e.dma_start(out=XS[:, i, :], in_=XL[tix * 128:(tix + 1) * 128, :]),
                      reads=["XL"], writes=[("XS", i)], dma=True)
                if C.lvl < 2:
                    continue
                norm_tile(C, NB, ident, XS[:, i, :], ("XS", i), gm[:, r, :], sh2[:, r, :], ["gmv", k2],
                          lambda k, i=i: fT[:, k, i * 128:(i + 1) * 128], "fT",
                          dst_f32=lambda k, s=s: f32t[s][:, k, :], dst32_key=("f32t", s))
                if C.lvl < 3:
                    continue
                for k in range(8):
                    P.add("pe", lambda e, s=s, k=k: e.matmul(pl[s][:, 0:NE], f32t[s][:, k, :], wr[:, k, :],
                                                             start=(k == 0), stop=(k == 7)),
                          reads=[("f32t", s), "wr"], writes=[("pl", s)])
                P.add("dve", lambda e, s=s: e.tensor_tensor(lg[s][:], pl[s][:, 0:NE], brb[:], ALU.add),
                      reads=[("pl", s), "brb"], writes=[("lg", s)])
                P.add("dve", lambda e, s=s: e.max(m8[s][:], lg[s][:]), reads=[("lg", s)], writes=[("m8", s)])
                P.add("dve", lambda e, s=s: e.tensor_scalar(nm[s][:], m8[s][:, 0:1], -1.0, None, ALU.mult),
                      reads=[("m8", s)], writes=[("nm", s)])
                P.add("act", lambda e, s=s: e.activation(ex[s][:], lg[s][:], AF.Exp, bias=nm[s][:, 0:1], scale=1.0),
                      reads=[("lg", s), ("nm", s)], writes=[("ex", s)])
                P.add("dve", lambda e, s=s: e.tensor_scalar(mk[s][:], lg[s][:], m8[s][:, 3:4], None, ALU.is_ge),
                      reads=[("lg", s), ("m8", s)], writes=[("mk", s)])
                P.add("dve", lambda e, s=s: e.tensor_tensor(ex[s][:], ex[s][:], mk[s][:], ALU.mult),
                      reads=[("ex", s), ("mk", s)], writes=[("ex", s)])
                P.add("dve", lambda e, s=s: e.reduce_sum(sm[s][:], ex[s][:], AX.X), reads=[("ex", s)], writes=[("sm", s)])
                P.add("dve", lambda e, s=s: e.reciprocal(sm[s][:], sm[s][:]), reads=[("sm", s)], writes=[("sm", s)])
                P.add("dve", lambda e, s=s, i=i: e.tensor_scalar(gates[:, i, :], ex[s][:], sm[s][:, 0:1], None, ALU.mult),
                      reads=[("ex", s), ("sm", s)], writes=[("gates", i)])
                if C.ne == NE:
                    P.add("pe", lambda e, s=s, i=i: e.transpose(pl[s][0:NE, 128:256], gates[:, i, :], ident[:]),
                          reads=[("gates", i), "ident"], writes=[("pl", s)])
                    P.add("act", lambda e, s=s: e.activation(gT[s][:], pl[s][0:NE, 128:256], AF.Copy), reads=[("pl", s)], writes=[("gT", s)])
                    for hh in range(2):
                        b_ = bi_ % 2
                        bi_ += 1
                        P.add("pe", lambda e, s=s, b_=b_, hh=hh: e.matmul(pbias[b_][:], gT[s][:], bdall[:, hh * 512:(hh + 1) * 512], start=True, stop=True),
                              reads=[("gT", s), "bdall"], writes=[("pbias", b_)])
                        P.add("dve", lambda e, b_=b_, r=r, hh=hh: e.tensor_tensor(btmp[b_][:], pbias[b_][:], g2p[:, r, hh * 512:(hh + 1) * 512], ALU.mult),
                              reads=[("pbias", b_), "g2p"], writes=[("btmp", b_)])
                        P.add("pool", lambda e, b_=b_, i=i, hh=hh: e.tensor_tensor(XS[:, i, hh * 512:(hh + 1) * 512], XS[:, i, hh * 512:(hh + 1) * 512],
                                                                                    btmp[b_][:], ALU.add),
                              reads=[("btmp", b_), ("XS", i)], writes=[("XS", i)])
        P.barrier()
        import os
        if "nopre" in os.environ.get("SK", ""):
            return
        with ExitStack() as st3:
            NG = 4
            wgu = [C.sb(st3, "wgu", [128, 2, 8, 128], BF16) for _ in range(NG)]
            wdn = [C.sb(st3, "wdn", [128, 8, 1024], BF16) for _ in range(2)]
            ones1 = C.sb(st3, "ones1", [1, 128], F32)
            bgu = C.sb(st3, "bgu", [128, NE, 16], F32)
            g2b = C.sb(st3, "g2b", [128, 2, 1024], F32)
            actT = C.sb(st3, "actT", [128, 8, NTOK], BF16)
            gc = [C.sb(st3, "gc", [128, 512], F32) for _ in range(2)]
            sgm = [C.sb(st3, "sgm", [128, 512], F32) for _ in range(2)]
            u1 = [C.sb(st3, "u1", [128, 512], F32) for _ in range(2)]
            yt = [C.sb(st3, "yt", [128, 512], F32) for _ in range(2)]
            yt2 = [C.sb(st3, "yt2", [128, 512], F32) for _ in range(2)]
            pg = [C.ps(st3, "pg", [128, 512]) for _ in range(2)]
            pu = [C.ps(st3, "pu", [128, 512]) for _ in range(2)]
            py = [C.ps(st3, "py", [128, 512]) for _ in range(3)]
            for s_ in range(3):
                C.pskey(("pg", s_)); C.pskey(("pu", s_)); C.pskey(("py", s_))
            import os
            SK = os.environ.get("SK", "")
            if "ones1" not in SK:
                P.add("pool", lambda e: e.memset(ones1[:], 1.0), writes=["ones1"])
            if "bguld" not in SK:
                P.add("sp", lambda e: e.dma_start(out=bgu[:].rearrange("p e j -> p (e j)"), in_=io["b_guT"][layer]), writes=["bgu"], dma=True)
            import os
            SK = os.environ.get("SK", "")
            if "bguadd" not in SK:
                P.add("dve", lambda e: e.tensor_scalar(bgu[:, :, 8:16], bgu[:, :, 8:16], 1.0, None, ALU.add),
                      reads=["bgu"], writes=["bgu"])
            for r in range(2):
                if "g2b" in SK:
                    continue
                src = io["modrow"][layer, r:r + 1, 5 * 1024:6 * 1024].partition_broadcast(128)
                P.add("sp", lambda e, r=r, src=src: e.dma_start(out=g2b[:, r, :], in_=src), reads=["modrow"],
                      writes=["g2b"], dma=True)
            NS = 4
            wstg = [C.sb(st3, "wstg", [128, 2048], F32) for _ in range(NS)]
            nea = io["nea"]
            items = []
            for ex_ in range(C.ne):
                order = [("gu", 0), ("gu", 1), ("dn", 0), ("gu", 2), ("dn", 1), ("gu", 3), ("dn", 2), ("gu", 4), ("dn", 3),
                         ("gu", 5), ("gu", 6), ("gu", 7)]
                items += [(ex_, kind, q) for (kind, q) in order]
            pos = {it_: n_ for n_, it_ in enumerate(items)}
            state = {"staged": 0, "sti": 0, "ci": 0}
            slot_of = {}

            def stage_upto(n_):
                while state["staged"] <= min(n_, len(items) - 1):
                    ex2, kind, q = items[state["staged"]]
                    state["staged"] += 1
                    if C.nowdma and ex2 >= 2:
                        if kind == "gu":
                            slot_of[(ex2, q)] = state["ci"] % NG
                            state["ci"] += 1
                        continue
                    sg_ = state["sti"] % NS
                    state["sti"] += 1
                    if kind == "gu":
                        cs2 = state["ci"] % NG
                        state["ci"] += 1
                        slot_of[(ex2, q)] = cs2
                        src = io["w_guR"][(layer * nea + ex2) * 8 + q]
                        P.add("sp", lambda e, sg_=sg_, src=src: e.dma_start(out=wstg[sg_][:], in_=src), writes=[("wstg", sg_)], dma=True)
                        P.add("act", lambda e, sg_=sg_, cs2=cs2: e.activation(wgu[cs2][:].rearrange("p u k c -> p (u k c)"), wstg[sg_][:], AF.Copy),
                              reads=[("wstg", sg_)], writes=[("wgu", cs2)])
                    else:
                        ws2 = ex2 % 2
                        src = io["w_down"][layer * nea + ex2].rearrange("(j p) d -> p j d", p=128)[:, 2 * q:2 * q + 2, :]
                        P.add("sp", lambda e, sg_=sg_, src=src: e.dma_start(out=wstg[sg_][:].rearrange("p (j d) -> p j d", j=2), in_=src),
                              writes=[("wstg", sg_)], dma=True)
                        P.add("act", lambda e, sg_=sg_, ws2=ws2, q=q: e.activation(
                            wdn[ws2][:, 2 * q:2 * q + 2, :].rearrange("p j d -> p (j d)"), wstg[sg_][:], AF.Copy),
                            reads=[("wstg", sg_)], writes=[("wdn", ws2, q)])

            si = 0
            yi = 0
            pending_tail = []
            for ex_ in range(C.ne):
                ws = ex_ % 2
                for j in range(8):
                    stage_upto(pos[(ex_, "gu", j)] + 3)
                    cs_ = slot_of[(ex_, j)]
                    for (t0, nt) in groups:
                        w = nt * 128
                        c0 = t0 * 128
                        ss_ = si % 2
                        si += 1
                        for k in range(8):
                            P.add("pe", lambda e, cs_=cs_, k=k, ss_=ss_, c0=c0, w=w: e.matmul(
                                pg[ss_][:, 0:w], wgu[cs_][:, 0, k, :], fT[:, k, c0:c0 + w], start=(k == 0), stop=(k == 7)),
                                reads=[("wgu", cs_), "fT"], writes=[("pg", ss_)])
                        for k in range(8):
                            P.add("pe", lambda e, cs_=cs_, k=k, ss_=ss_, c0=c0, w=w: e.matmul(
                                pu[ss_][:, 0:w], wgu[cs_][:, 1, k, :], fT[:, k, c0:c0 + w], start=(k == 0), stop=(k == 7)),
                                reads=[("wgu", cs_), "fT"], writes=[("pu", ss_)])
                        P.add("dve", lambda e, ss_=ss_, w=w, ex_=ex_, j=j: e.tensor_scalar(
                            gc[ss_][:, 0:w], pg[ss_][:, 0:w], bgu[:, ex_, j:j + 1], 7.0, ALU.add, ALU.min),
                            reads=[("pg", ss_), "bgu"], writes=[("gc", ss_)])
                        P.add("act", lambda e, ss_=ss_, w=w: e.activation(sgm[ss_][:, 0:w], gc[ss_][:, 0:w], AF.Sigmoid, scale=1.702),
                              reads=[("gc", ss_)], writes=[("sgm", ss_)])
                        P.add("act", lambda e, ss_=ss_, w=w, ex_=ex_, j=j: e.activation(
                            u1[ss_][:, 0:w], pu[ss_][:, 0:w], AF.Identity, bias=bgu[:, ex_, 8 + j:9 + j], scale=1.0),
                            reads=[("pu", ss_), "bgu"], writes=[("u1", ss_)])
                        def tail(ss_=ss_, w=w, j=j, c0=c0):
                            P.add("pool", lambda e: e.tensor_tensor(sgm[ss_][:, 0:w], gc[ss_][:, 0:w], sgm[ss_][:, 0:w], ALU.mult),
                                  reads=[("gc", ss_), ("sgm", ss_)], writes=[("sgm", ss_)])
                            P.add("dve", lambda e: e.tensor_scalar(u1[ss_][:, 0:w], u1[ss_][:, 0:w], -6.0, 8.0, ALU.max, ALU.min),
                                  reads=[("u1", ss_)], writes=[("u1", ss_)])
                            P.add("dve", lambda e: e.tensor_tensor(actT[:, j, c0:c0 + w], sgm[ss_][:, 0:w], u1[ss_][:, 0:w], ALU.mult),
                                  reads=[("sgm", ss_), ("u1", ss_)], writes=[("actT", j)])
                        if pending_tail:
                            pending_tail.pop()()
                        pending_tail.append(tail)
                if pending_tail:
                    pending_tail.pop()()
                for i, tix in enumerate(tiles):
                    r = 1 if tix in ctx_tiles else 0
                    for hh in range(2):
                        ys = yi % 3
                        y2 = yi % 2
                        yi += 1
                        for j in range(8):
                            P.add("pe", lambda e, ys=ys, j=j, i=i, ws=ws, hh=hh: e.matmul(
                                py[ys][:], actT[:, j, i * 128:(i + 1) * 128], wdn[ws][:, j, hh * 512:(hh + 1) * 512],
                                start=(j == 0), stop=(j == 7)),
                                reads=[("actT", j), ("wdn", ws, j // 2)], writes=[("py", ys)])
                        P.add("act", lambda e, ys=ys, y2=y2, i=i, ex_=ex_: e.activation(
                            yt[y2][:], py[ys][:], AF.Copy, scale=gates[:, i, ex_:ex_ + 1]),
                            reads=[("py", ys), "gates"], writes=[("yt", y2)])
                        P.add("dve", lambda e, y2=y2, r=r, hh=hh: e.tensor_tensor(
                            yt2[y2][:], yt[y2][:], g2b[:, r, hh * 512:(hh + 1) * 512], ALU.mult),
                            reads=[("yt", y2), "g2b"], writes=[("yt2", y2)])
                        P.add("pool", lambda e, y2=y2, i=i, hh=hh: e.tensor_tensor(
                            XS[:, i, hh * 512:(hh + 1) * 512], XS[:, i, hh * 512:(hh + 1) * 512], yt2[y2][:], ALU.add),
                            reads=[("yt2", y2), ("XS", i, hh)], writes=[("XS", i, hh)])
            for i, tix in enumerate(tiles):
                if "wb" in SK:
                    continue
                if out_ap is not None:
                    if tix not in out_tiles:
                        continue
                    o = out_tiles[tix]
                    dst = out_ap[o * 128:(o + 1) * 128, :]
                    wkey = "OUT"
                else:
                    dst = XL[tix * 128:(tix + 1) * 128, :]
                    wkey = "XL"
                P.add("sp", lambda e, i=i, dst=dst: e.dma_start(out=dst, in_=XS[:, i, :]),
                      reads=[("XS", i, 0), ("XS", i, 1), ("XS", i)], writes=[wkey], dma=True)
    P.barrier()


def col_groups(n, w=512):
    out = []
    c = 0
    while c < n:
        out.append((c, min(w, n - c)))
        c += w
    return out


def mm_acc(C, out_ap, pskey, pairs, reads):
    n = len(pairs)
    for i, (l, r) in enumerate(pairs):
        C.P.add("pe", lambda e, l=l, r=r, i=i: e.matmul(out_ap, l, r, start=(i == 0), stop=(i == n - 1)),
                reads=reads, writes=[pskey])


def evac(C, eng, out_ap, in_ap, reads, writes):
    if eng == "act":
        C.P.add("act", lambda e: e.activation(out_ap, in_ap, AF.Copy), reads=reads, writes=writes)
    else:
        C.P.add(eng, lambda e: e.tensor_copy(out_ap, in_ap), reads=reads, writes=writes)


def rstd_from_ss(C, rs, ss, n, key_ss, key_rs):
    C.P.add("act", lambda e: e.activation(rs, ss, AF.Sqrt, bias=EPS, scale=1.0 / n), reads=[key_ss], writes=[key_rs])
    C.P.add("dve", lambda e: e.reciprocal(rs, rs), reads=[key_rs], writes=[key_rs])


def phase_norm_all(C, io, st, layer, src, ntiles, ctx_tiles, hT, copy_to_xl):
    P = C.P
    ident = C.sb(st, "ident", [128, 128], F32)
    P.add("sp", lambda e: e.dma_start(out=ident[:], in_=io["ident"]), writes=["ident"], dma=True)
    with ExitStack() as st2:
        NB = NormBufs(C, st2, C.name("nb"))
        sc1, k1 = load_modcols(C, st2, io, layer, 1)
        sh1, k2 = load_modcols(C, st2, io, layer, 0)
        nm = C.sb(st2, "nm", [128, 8], F32)
        gm = C.sb(st2, "gm", [128, 2, 8], F32)
        P.add("sp", lambda e: e.dma_start(out=nm[:], in_=io["normT"][:, (2 * layer) * 8:(2 * layer + 1) * 8]),
              writes=["nm"], dma=True)
        for r in range(2):
            P.add("dve", lambda e, r=r: e.scalar_tensor_tensor(gm[:, r, :], sc1[:, r, :], 1.0, nm[:], ALU.add, ALU.mult),
                  reads=[k1, "nm"], writes=["gm1"])
        xt = [C.sb(st2, "xt", [128, 1024], F32) for _ in range(2)]
        for t in range(ntiles):
            s = t % 2
            r = 1 if t in ctx_tiles else 0
            P.add("sp", lambda e, s=s, t=t: e.dma_start(out=xt[s][:], in_=src[t * 128:(t + 1) * 128, :]),
                  reads=["SRC"], writes=[("xt", s)], dma=True)
            if copy_to_xl and t < NT_OWN:
                P.add("sp", lambda e, s=s, t=t: e.dma_start(out=io["XL"][t * 128:(t + 1) * 128, :], in_=xt[s][:]),
                      reads=[("xt", s)], writes=["XL"], dma=True)
            norm_tile(C, NB, ident, xt[s][:], ("xt", s), gm[:, r, :], sh1[:, r, :], ["gm1", k2],
                      lambda k, t=t: hT[:, k, t * 128:(t + 1) * 128], "hT")
    P.barrier()
    return ident


def phase_inproj0(C, io):
    P = C.P
    with ExitStack() as st:
        hT = C.sb(st, "hT", [128, 8, NALL], BF16)
        phase_norm_all(C, io, st, 0, io["xall"], NT_ALL, {0, 1}, hT, True)
        win = C.sb(st, "win", [128, 8, 1440], BF16)
        wsrc = io["w_in"].rearrange("(k p) f -> p k f", p=128)
        for k in range(8):
            P.add("pool", lambda e, k=k: e.dma_start(out=win[:, k, :], in_=wsrc[:, k, :]), writes=["win"], dma=True)
        stg = [C.sb(st, "stg", [128, 512], F32) for _ in range(3)]
        pp = [C.ps(st, "pp", [128, 512]) for _ in range(3)]
        for s_ in range(3):
            C.pskey(("pp", s_))
        it = 0
        for (f0, ncols, dst) in ((0, NALL, io["xaT"]), (512, NOWN, io["gaT"])):
            for kc in range(4):
                for (c0, w) in col_groups(ncols):
                    s = it % 3
                    it += 1
                    mm_acc(C, pp[s][:, 0:w], ("pp", s),
                           [(win[:, k, f0 + kc * 128:f0 + (kc + 1) * 128], hT[:, k, c0:c0 + w]) for k in range(8)],
                           ["win", "hT"])
                    evac(C, "act" if it % 2 else "dve", stg[s][:, 0:w], pp[s][:, 0:w], [("pp", s)], [("stg", s)])
                    P.add("sp", lambda e, s=s, w=w, c0=c0, kc=kc, dst=dst: e.dma_start(
                        out=dst[kc * 128:(kc + 1) * 128, c0:c0 + w], in_=stg[s][:, 0:w]),
                        reads=[("stg", s)], writes=["fm_out"], dma=True)
        for t in range(NT_ALL):
            s = it % 3
            it += 1
            mm_acc(C, pp[s][:, 0:416], ("pp", s),
                   [(hT[:, k, t * 128:(t + 1) * 128], win[:, k, 1024:1440]) for k in range(8)], ["win", "hT"])
            evac(C, "act" if it % 2 else "dve", stg[s][:, 0:416], pp[s][:, 0:416], [("pp", s)], [("stg", s)])
            P.add("sp", lambda e, s=s, t=t: e.dma_start(out=io["qkr"][t * 128:(t + 1) * 128, :], in_=stg[s][:, 0:416]),
                  reads=[("stg", s)], writes=["qkr"], dma=True)
    P.barrier()


def phase_lru(C, io):
    P = C.P
    SEG = ((0, NCTX), (NCTX, NALL))
    with ExitStack() as st:
        lv = C.sb(st, "lv", [128, 2, 4, 8], F32)
        cst = C.sb(st, "cst", [128, 2, 4, 2], F32)
        wbd = C.sb(st, "wbd", [128, 16, 128], F32)
        P.add("sp", lambda e: e.dma_start(out=lv[:].rearrange("p a b c -> p (a b c)"), in_=io["lruvec"]), writes=["lv"], dma=True)
        P.add("sp", lambda e: e.dma_start(out=wbd[:], in_=io["wbd"].rearrange("n c d -> c n d")), writes=["wbd"], dma=True)
        P.add("act", lambda e: e.activation(cst[:, :, :, 0], lv[:, :, :, 7], AF.Exp, scale=-1.0), reads=["lv"], writes=["cst"])
        P.add("act", lambda e: e.activation(cst[:, :, :, 0], cst[:, :, :, 0], AF.Ln, bias=1.0), reads=["cst"], writes=["cst"])
        P.add("dve", lambda e: e.tensor_scalar(cst[:, :, :, 1], cst[:, :, :, 0], -16.0, None, ALU.mult), reads=["cst"], writes=["cst2"])
        P.add("dve", lambda e: e.tensor_scalar(cst[:, :, :, 0], cst[:, :, :, 0], -8.0, None, ALU.mult), reads=["cst", "cst2"], writes=["cst"])
        xa = C.sb(st, "xa", [128, NALL], F32)
        xc = C.sb(st, "xc", [128, NALL], F32)
        ra = C.sb(st, "ra", [128, NALL], F32)
        gb = C.sb(st, "gb", [128, NALL], F32)
        e2 = C.sb(st, "e2", [128, NALL], F32)
        hh_ = [C.sb(st, "hf", [128, NALL], F32), C.sb(st, "hb", [128, NALL], F32)]
        ga = C.sb(st, "ga", [128, NOWN], F32)
        ya = C.sb(st, "ya", [128, NOWN], BF16)
        pr = [C.ps(st, "pr", [128, 512]) for _ in range(2)]
        pi = [C.ps(st, "pi", [128, 512]) for _ in range(2)]
        for s_ in range(2):
            C.pskey(("pr", s_))
            C.pskey(("pi", s_))
        it = 0
        for kc in range(4):
            P.add("sp", lambda e, kc=kc: e.dma_start(out=xa[:], in_=io["xaT"][kc * 128:(kc + 1) * 128, :]), writes=["xa"], dma=True)
            P.add("sp", lambda e, kc=kc: e.dma_start(out=ga[:], in_=io["gaT"][kc * 128:(kc + 1) * 128, :]), writes=["ga"], dma=True)
            for dl in range(2):
                V = lambda f, dl=dl, kc=kc: lv[:, dl, kc, f:f + 1]
                P.add("dve", lambda e, V=V: e.tensor_scalar(xc[:], xa[:], V(3), V(4), ALU.mult, ALU.add),
                      reads=["xa", "lv"], writes=["xc"])
                for j in range(3):
                    sft = 3 - j
                    for (a, b) in SEG:
                        if dl == 0:
                            o_sl, i_sl = (a + sft, b), (a, b - sft)
                        else:
                            o_sl, i_sl = (a, b - sft), (a + sft, b)
                        P.add("dve", lambda e, V=V, j=j, o_sl=o_sl, i_sl=i_sl: e.scalar_tensor_tensor(
                            xc[:, o_sl[0]:o_sl[1]], xa[:, i_sl[0]:i_sl[1]], V(j), xc[:, o_sl[0]:o_sl[1]], ALU.mult, ALU.add),
                            reads=["xa", "lv", "xc"], writes=["xc"])
                for (c0, w) in col_groups(NALL):
                    s = it % 2
                    it += 1
                    P.add("pe", lambda e, s=s, c0=c0, w=w, dl=dl, kc=kc: e.matmul(pr[s][:, 0:w], wbd[:, (0 * 2 + dl) * 4 + kc, :], xc[:, c0:c0 + w],
                                                                                start=True, stop=True),
                          reads=["wbd", "xc"], writes=[("pr", s)])
                    P.add("pe", lambda e, s=s, c0=c0, w=w, dl=dl, kc=kc: e.matmul(pi[s][:, 0:w], wbd[:, (1 * 2 + dl) * 4 + kc, :], xc[:, c0:c0 + w],
                                                                                start=True, stop=True),
                          reads=["wbd", "xc"], writes=[("pi", s)])
                    P.add("act", lambda e, s=s, c0=c0, w=w, V=V: e.activation(ra[:, c0:c0 + w], pr[s][:, 0:w], AF.Sigmoid, bias=V(5)),
                          reads=[("pr", s), "lv"], writes=["ra"])
                    P.add("act", lambda e, s=s, c0=c0, w=w, V=V: e.activation(gb[:, c0:c0 + w], pi[s][:, 0:w], AF.Sigmoid, bias=V(6)),
                          reads=[("pi", s), "lv"], writes=["gb"])
                cc = lambda f, dl=dl, kc=kc: cst[:, dl, kc, f:f + 1]
                P.add("act", lambda e, cc=cc: e.activation(e2[:], ra[:], AF.Exp, scale=cc(1)), reads=["ra", "cst", "cst2"], writes=["e2"])
                P.add("act", lambda e, cc=cc: e.activation(ra[:], ra[:], AF.Exp, scale=cc(0)), reads=["ra", "cst", "cst2", "e2"], writes=["ra"])
                P.add("act", lambda e: e.activation(e2[:], e2[:], AF.Sqrt, bias=1.0, scale=-1.0), reads=["e2"], writes=["e2"])
                P.add("dve", lambda e: e.tensor_tensor(gb[:], gb[:], xc[:], ALU.mult), reads=["gb", "xc"], writes=["gb"])
                P.add("pool", lambda e: e.tensor_tensor(gb[:], gb[:], e2[:], ALU.mult), reads=["gb", "e2"], writes=["gb"])
                h = hh_[dl]
                hk = ("h", dl)
                if dl == 0:
                    P.add("dve", lambda e, h=h: e.tensor_tensor_scan(h[:], ra[:], gb[:], 0.0, ALU.mult, ALU.add),
                          reads=["ra", "gb"], writes=[hk])
                else:
                    P.add("dve", lambda e, h=h: e.tensor_tensor_scan(h[:, NCTX - 1::-1], ra[:, NCTX - 1::-1], gb[:, NCTX - 1::-1], 0.0,
                                                                     ALU.mult, ALU.add),
                          reads=["ra", "gb"], writes=[hk])
                    P.add("dve", lambda e, h=h: e.tensor_tensor_scan(h[:, NALL - 1:NCTX - 1:-1], ra[:, NALL - 1:NCTX - 1:-1],
                                                                     gb[:, NALL - 1:NCTX - 1:-1], h[:, 0:1], ALU.mult, ALU.add),
                          reads=["ra", "gb", hk], writes=[hk])
            hf, hb = hh_
            P.add("pool", lambda e: e.tensor_tensor(hf[:, 0:NOWN], hf[:, 0:NOWN], hb[:, 0:NOWN], ALU.add),
                  reads=[("h", 0), ("h", 1)], writes=[("h", 0)])
            t1 = xc[:, 0:NOWN]
            P.add("dve", lambda e: e.tensor_tensor(t1, ga[:], ga[:], ALU.mult), reads=["ga", "xc"], writes=["xc"])
            P.add("dve", lambda e: e.tensor_scalar(t1, t1, 0.044715, 1.0, ALU.mult, ALU.add), reads=["xc"], writes=["xc"])
            P.add("dve", lambda e: e.tensor_tensor(t1, t1, ga[:], ALU.mult), reads=["xc", "ga"], writes=["xc"])
            P.add("act", lambda e: e.activation(t1, t1, AF.Sigmoid, scale=1.5957691216057308), reads=["xc"], writes=["xc"])
            P.add("dve", lambda e: e.tensor_tensor(t1, t1, ga[:], ALU.mult), reads=["xc", "ga"], writes=["xc"])
            P.add("dve", lambda e: e.tensor_tensor(ya[:], t1, hf[:, 0:NOWN], ALU.mult), reads=["xc", ("h", 0)], writes=["ya"])
            P.add("sp", lambda e, kc=kc: e.dma_start(out=io["mixT"][kc * 128:(kc + 1) * 128, :], in_=ya[:]),
                  reads=["ya"], writes=["mixT"], dma=True)
    P.barrier()


def rope_apply(C, out, x, cs, tmp, G, h, reads, wkey, tkey):
    P = C.P
    cosb = cs[:, 0:h].unsqueeze(1).broadcast_to([128, G, h])
    sinb = cs[:, h:2 * h].unsqueeze(1).broadcast_to([128, G, h])
    x1, x2 = x[:, :, 0:h], x[:, :, h:2 * h]
    o1, o2 = out[:, :, 0:h], out[:, :, h:2 * h]
    t1, t2 = tmp[:, :, 0:h], tmp[:, :, h:2 * h]
    P.add("dve", lambda e: e.tensor_tensor(t1, x2, sinb, ALU.mult), reads=reads, writes=[tkey])
    P.add("dve", lambda e: e.tensor_tensor(t2, x1, sinb, ALU.mult), reads=reads, writes=[tkey])
    P.add("dve", lambda e: e.tensor_tensor(o1, x1, cosb, ALU.mult), reads=reads, writes=[wkey])
    P.add("dve", lambda e: e.tensor_tensor(o2, x2, cosb, ALU.mult), reads=reads, writes=[wkey])
    P.add("dve", lambda e: e.tensor_tensor(o1, o1, t1, ALU.subtract), reads=[wkey, tkey], writes=[wkey])
    P.add("dve", lambda e: e.tensor_tensor(o2, o2, t2, ALU.add), reads=[wkey, tkey], writes=[wkey])


def phase_mla_prep(C, io):
    P = C.P
    with ExitStack() as st:
        ident = C.sb(st, "ident", [128, 128], F32)
        P.add("sp", lambda e: e.dma_start(out=ident[:], in_=io["ident"]), writes=["ident"], dma=True)
        wkvb = C.sb(st, "wkvb", [128, 1024], BF16)
        wqb = C.sb(st, "wqb", [128, 2, 768], BF16)
        P.add("pool", lambda e: e.dma_start(out=wkvb[:], in_=io["w_kv_b"]), writes=["wkvb"], dma=True)
        P.add("pool", lambda e: e.dma_start(out=wqb[:], in_=io["w_q_b"].rearrange("(c p) f -> p c f", p=128)), writes=["wqb"], dma=True)
        qan = C.sb(st, "qan", [128, 2], F32)
        kvan = C.sb(st, "kvan", [128, 1], F32)
        P.add("sp", lambda e: e.dma_start(out=qan[:], in_=io["q_a_normT"]), writes=["nrm"], dma=True)
        P.add("sp", lambda e: e.dma_start(out=kvan[:], in_=io["kv_a_normT"]), writes=["nrm"], dma=True)
        nn = C.sb(st, "nn", [128, 2, 64], F32)
        rn = C.sb(st, "rn", [128, 2, 32], F32)
        for i in range(2):
            P.add("sp", lambda e, i=i: e.dma_start(out=nn[:, i, :], in_=io["nope_norm"][i:i + 1, :].partition_broadcast(128)), writes=["nrm"], dma=True)
            P.add("sp", lambda e, i=i: e.dma_start(out=rn[:, i, :], in_=io["rope_norm"][i:i + 1, :].partition_broadcast(128)), writes=["nrm"], dma=True)
        gqk = C.sb(st, "gqk", [128, 64], F32)
        P.add("dve", lambda e: e.tensor_tensor(gqk[:], nn[:, 0, :], nn[:, 1, :], ALU.mult), reads=["nrm"], writes=["gqk"])
        junk = C.sb(st, "junk", [128, 256], F32)
        B2 = lambda nm_, shp, dt: [C.sb(st, nm_, shp, dt) for _ in range(2)]
        qk = B2("qk", [128, 416], F32)
        cs = B2("cs", [128, 32], F32)
        ssv = B2("ssv", [128, 4], F32)
        rsv = B2("rsv", [128, 4], F32)
        kvn = B2("kvn", [128, 128], F32)
        kvnT = B2("kvnT", [128, 128], BF16)
        sq = B2("sq", [128, 4, 96], F32)
        ssn = B2("ssn", [128, 24], F32)
        rsn = B2("rsn", [128, 24], F32)
        kcat = B2("kcat", [128, 8, 96], F32)
        qcat = B2("qcat", [128, 8, 96], F32)
        vaug = B2("vaug", [128, 8, 65], BF16)
        kr = B2("kr", [128, 1, 32], F32)
        kr2 = B2("kr2", [128, 1, 32], F32)
        rtmp = B2("rtmp", [128, 8, 32], F32)
        qr = B2("qr", [128, 8, 32], F32)
        qn = B2("qn", [128, 256], F32)
        qnT = B2("qnT", [128, 2, 128], BF16)
        ktile = B2("ktile", [96, 8, 128], BF16)
        qtile = B2("qtile", [96, 8, 128], BF16)
        pT = [C.ps(st, "pT", [128, 512]) for _ in range(1)]
        pkv = [C.ps(st, "pkv", [128, 512]) for _ in range(2)]
        pq = [C.ps(st, "pq", [128, 512]) for _ in range(2)]
        pT2 = [C.ps(st, "pT2", [128, 512]) for _ in range(2)]
        for s_ in range(2):
            C.pskey(("pT", s_)); C.pskey(("pkv", s_)); C.pskey(("pq", s_)); C.pskey(("pT2", s_))
        for s_ in range(2):
            P.add("pool", lambda e, s_=s_: e.memset(vaug[s_][:], 1.0), writes=[("vaug", s_)])
        KTd = io["KT"].rearrange("h f t -> f h t")
        QTd = io["QT"].rearrange("h f t -> f h t")
        for t in range(NT_ALL):
            s = t % 2
            K = lambda n_, s=s: (n_, s)
            lat = t >= 2
            own = t < NT_OWN
            P.add("sp", lambda e, s=s, t=t: e.dma_start(out=qk[s][:], in_=io["qkr"][t * 128:(t + 1) * 128, :]), writes=[K("qk")], dma=True)
            if lat:
                P.add("sp", lambda e, s=s, t=t: e.dma_start(out=cs[s][:], in_=io["rope_mla"][(t - 2) * 128:(t - 1) * 128, :]),
                      writes=[K("cs")], dma=True)
            P.add("act", lambda e, s=s: e.activation(junk[:, 0:128], qk[s][:, 256:384], AF.Square, accum_out=ssv[s][:, 0:1]),
                  reads=[K("qk")], writes=["junk", K("ssv")])
            P.add("act", lambda e, s=s: e.activation(junk[:, 0:32], qk[s][:, 384:416], AF.Square, accum_out=ssv[s][:, 1:2]),
                  reads=[K("qk")], writes=["junk", K("ssv")])
            if own:
                P.add("act", lambda e, s=s: e.activation(junk[:, 0:256], qk[s][:, 0:256], AF.Square, accum_out=ssv[s][:, 2:3]),
                      reads=[K("qk")], writes=["junk", K("ssv")])
            for (c, n_) in ((0, 128), (1, 32)) + (((2, 256),) if own else ()):
                rstd_from_ss(C, rsv[s][:, c:c + 1], ssv[s][:, c:c + 1], n_, K("ssv"), K("rsv"))
            P.add("dve", lambda e, s=s: e.tensor_scalar(kvn[s][:], qk[s][:, 256:384], rsv[s][:, 0:1], None, ALU.mult),
                  reads=[K("qk"), K("rsv")], writes=[K("kvn")])
            P.add("pe", lambda e, s=s: e.transpose(pT[0][:, 0:128], kvn[s][:], ident[:]), reads=[K("kvn"), "ident"], writes=[("pT", 0)])
            P.add("act", lambda e, s=s: e.activation(kvnT[s][:], pT[0][:, 0:128], AF.Copy, scale=kvan[:, 0:1]),
                  reads=[("pT", 0), "nrm"], writes=[K("kvnT")])
            for b in range(2):
                P.add("pe", lambda e, s=s, b=b: e.matmul(pkv[b][:], kvnT[s][:], wkvb[:, b * 512:(b + 1) * 512], start=True, stop=True),
                      reads=[K("kvnT"), "wkvb"], writes=[("pkv", b)])
                pv = pkv[b][:].rearrange("p (h c) -> p h c", h=4)
                P.add("act", lambda e, s=s, pv=pv: e.activation(sq[s][:, :, 0:64], pv[:, :, 0:64], AF.Square),
                      reads=[("pkv", b)], writes=[K("sq")])
                P.add("dve", lambda e, s=s, b=b: e.reduce_sum(ssn[s][:, 4 * b:4 * b + 4], sq[s][:, :, 0:64], AX.X),
                      reads=[K("sq")], writes=[K("ssn")])
                P.add("act", lambda e, s=s, b=b, pv=pv: e.activation(vaug[s][:, 4 * b:4 * b + 4, 0:64], pv[:, :, 64:128], AF.Copy),
                      reads=[("pkv", b)], writes=[K("vaug")])
            rstd_from_ss(C, rsn[s][:, 0:8], ssn[s][:, 0:8], 64, K("ssn"), K("rsn"))
            for b in range(2):
                pv = pkv[b][:].rearrange("p (h c) -> p h c", h=4)
                P.add("dve", lambda e, s=s, b=b, pv=pv: e.tensor_tensor(
                    kcat[s][:, 4 * b:4 * b + 4, 0:64], pv[:, :, 0:64],
                    rsn[s][:, 4 * b:4 * b + 4].unsqueeze(2).broadcast_to([128, 4, 64]), ALU.mult),
                    reads=[("pkv", b), K("rsn")], writes=[K("kcat")])
            P.add("dve", lambda e, s=s: e.tensor_scalar(kr[s][:, 0, :], qk[s][:, 384:416], rsv[s][:, 1:2], None, ALU.mult),
                  reads=[K("qk"), K("rsv")], writes=[K("kr")])
            P.add("dve", lambda e, s=s: e.tensor_tensor(kr[s][:, 0, :], kr[s][:, 0, :], rn[:, 1, :], ALU.mult),
                  reads=[K("kr"), "nrm"], writes=[K("kr")])
            if lat:
                rope_apply(C, kr2[s][:], kr[s][:], cs[s][:], rtmp[s][:, 0:1, :], 1, 16, [K("kr"), K("cs")], K("kr2"), K("rtmp"))
                ksrc, kkey = kr2[s], K("kr2")
            else:
                ksrc, kkey = kr[s], K("kr")
            P.add("pool", lambda e, s=s, ksrc=ksrc: e.tensor_copy(kcat[s][:, :, 64:96], ksrc[:, 0:1, :].broadcast_to([128, 8, 32])),
                  reads=[kkey], writes=[K("kcat")])
            for b in range(2):
                for hh in range(4):
                    h = 4 * b + hh
                    P.add("pe", lambda e, s=s, b=b, hh=hh, h=h: e.transpose(pT2[b][0:96, hh * 128:(hh + 1) * 128], kcat[s][:, h, :], ident[:]),
                          reads=[K("kcat"), "ident"], writes=[("pT2", b)])
                evac(C, "act" if b else "dve", ktile[s][:, 4 * b:4 * b + 4, :], pT2[b][0:96, :].rearrange("p (h c) -> p h c", h=4),
                     [("pT2", b)], [K("ktile")])
            P.add("sp", lambda e, s=s, t=t: e.dma_start(out=KTd[:, :, t * 128:(t + 1) * 128], in_=ktile[s][:]),
                  reads=[K("ktile")], writes=["KT"], dma=True)
            P.add("sp", lambda e, s=s, t=t: e.dma_start(out=io["Vd"][t * 128:(t + 1) * 128, :], in_=vaug[s][:].rearrange("p h c -> p (h c)")),
                  reads=[K("vaug")], writes=["Vd"], dma=True)
            if not own:
                continue
            P.add("dve", lambda e, s=s: e.tensor_scalar(qn[s][:], qk[s][:, 0:256], rsv[s][:, 2:3], None, ALU.mult),
                  reads=[K("qk"), K("rsv")], writes=[K("qn")])
            for c in range(2):
                P.add("pe", lambda e, s=s, c=c: e.transpose(pT[0][:, (c + 1) * 128:(c + 2) * 128], qn[s][:, c * 128:(c + 1) * 128], ident[:]),
                      reads=[K("qn"), "ident"], writes=[("pT", 0)])
            for c in range(2):
                P.add("act", lambda e, s=s, c=c: e.activation(qnT[s][:, c, :], pT[0][:, (c + 1) * 128:(c + 2) * 128], AF.Copy, scale=qan[:, c:c + 1]),
                      reads=[("pT", 0), "nrm"], writes=[K("qnT")])
            for b in range(2):
                mm_acc(C, pq[b][:, 0:384], ("pq", b), [(qnT[s][:, c, :], wqb[:, c, b * 384:(b + 1) * 384]) for c in range(2)],
                       [K("qnT"), "wqb"])
                pv = pq[b][:, 0:384].rearrange("p (h c) -> p h c", h=4)
                P.add("act", lambda e, s=s, pv=pv: e.activation(sq[s][:], pv, AF.Square), reads=[("pq", b)], writes=[K("sq")])
                P.add("dve", lambda e, s=s, b=b: e.reduce_sum(ssn[s][:, 8 + 4 * b:12 + 4 * b], sq[s][:, :, 0:64], AX.X),
                      reads=[K("sq")], writes=[K("ssn")])
                P.add("dve", lambda e, s=s, b=b: e.reduce_sum(ssn[s][:, 16 + 4 * b:20 + 4 * b], sq[s][:, :, 64:96], AX.X),
                      reads=[K("sq")], writes=[K("ssn")])
            rstd_from_ss(C, rsn[s][:, 8:16], ssn[s][:, 8:16], 64, K("ssn"), K("rsn"))
            rstd_from_ss(C, rsn[s][:, 16:24], ssn[s][:, 16:24], 32, K("ssn"), K("rsn"))
            for b in range(2):
                pv = pq[b][:, 0:384].rearrange("p (h c) -> p h c", h=4)
                P.add("dve", lambda e, s=s, b=b, pv=pv: e.tensor_tensor(
                    qcat[s][:, 4 * b:4 * b + 4, 0:64], pv[:, :, 0:64],
                    rsn[s][:, 8 + 4 * b:12 + 4 * b].unsqueeze(2).broadcast_to([128, 4, 64]), ALU.mult),
                    reads=[("pq", b), K("rsn")], writes=[K("qcat")])
                P.add("dve", lambda e, s=s, b=b, pv=pv: e.tensor_tensor(
                    qr[s][:, 4 * b:4 * b + 4, :], pv[:, :, 64:96],
                    rsn[s][:, 16 + 4 * b:20 + 4 * b].unsqueeze(2).broadcast_to([128, 4, 32]), ALU.mult),
                    reads=[("pq", b), K("rsn")], writes=[K("qr")])
            P.add("pool", lambda e, s=s: e.tensor_tensor(qcat[s][:, :, 0:64], qcat[s][:, :, 0:64],
                                                         gqk[:].unsqueeze(1).broadcast_to([128, 8, 64]), ALU.mult),
                  reads=[K("qcat"), "gqk"], writes=[K("qcat")])
            P.add("pool", lambda e, s=s: e.tensor_tensor(qr[s][:], qr[s][:], rn[:, 0, :].unsqueeze(1).broadcast_to([128, 8, 32]), ALU.mult),
                  reads=[K("qr"), "nrm"], writes=[K("qr")])
            if lat:
                rope_apply(C, qcat[s][:, :, 64:96], qr[s][:], cs[s][:], rtmp[s][:], 8, 16, [K("qr"), K("cs")], K("qcat"), K("rtmp"))
            else:
                P.add("pool", lambda e, s=s: e.tensor_copy(qcat[s][:, :, 64:96], qr[s][:]), reads=[K("qr")], writes=[K("qcat")])
            for b in range(2):
                for hh in range(4):
                    h = 4 * b + hh
                    P.add("pe", lambda e, s=s, b=b, hh=hh, h=h: e.transpose(pT2[b][0:96, hh * 128:(hh + 1) * 128], qcat[s][:, h, :], ident[:]),
                          reads=[K("qcat"), "ident"], writes=[("pT2", b)])
                evac(C, "act" if b else "dve", qtile[s][:, 4 * b:4 * b + 4, :], pT2[b][0:96, :].rearrange("p (h c) -> p h c", h=4),
                     [("pT2", b)], [K("qtile")])
            P.add("sp", lambda e, s=s, t=t: e.dma_start(out=QTd[:, :, t * 128:(t + 1) * 128], in_=qtile[s][:]),
                  reads=[K("qtile")], writes=["QT"], dma=True)
    P.barrier()


def phase_mla_attn(C, io):
    P = C.P
    scale = 96.0 ** -0.5
    with ExitStack() as st:
        QTs = C.sb(st, "QTs", [96, 8, NOWN], BF16)
        P.add("sp", lambda e: e.dma_start(out=QTs[:], in_=io["QT"].rearrange("h f t -> f h t")), writes=["QTs"], dma=True)
        KTh = [C.sb(st, "KTh", [96, NALL], BF16) for _ in range(2)]
        Vall = C.sb(st, "Vall", [128, NT_ALL, 520], BF16)
        Vsrc = io["Vd"].rearrange("(t p) c -> p t c", p=128)
        for t0 in range(0, NT_ALL, 9):
            t1 = min(NT_ALL, t0 + 9)
            P.add("sp", lambda e, t0=t0, t1=t1: e.dma_start(out=Vall[:, t0:t1, :], in_=Vsrc[:, t0:t1, :]), writes=["Vall"], dma=True)
        ones65 = C.sb(st, "ones65", [65, 64], F32)
        P.add("pool", lambda e: e.memset(ones65[:], 1.0), writes=["ones65"])
        pt = [C.sb(st, "ptx", [128, 512], BF16) for _ in range(3)]
        rden = [C.sb(st, "rden", [65, 512], F32) for _ in range(2)]
        bcs = [C.sb(st, "bcs", [64, 512], F32) for _ in range(2)]
        ybt = [C.sb(st, "ybt", [64, 512], BF16) for _ in range(2)]
        ps_s = [C.ps(st, "ps_s", [128, 512]) for _ in range(3)]
        po = [C.ps(st, "po", [128, 512]) for _ in range(2)]
        pb = [C.ps(st, "pb", [128, 512]) for _ in range(2)]
        for s_ in range(3):
            C.pskey(("ps_s", s_)); C.pskey(("po", s_)); C.pskey(("pb", s_))
        qgroups = [(0, NCTX, [0, 1])] + [(NCTX + c0, w, list(range(NT_ALL))) for (c0, w) in col_groups(NOWN - NCTX)]
        si = 0
        gi = 0
        for h in range(8):
            hs = h % 2
            P.add("sp", lambda e, h=h, hs=hs: e.dma_start(out=KTh[hs][:], in_=io["KT"][h]), writes=[("KTh", hs)], dma=True)
            for (c0, w, kts) in qgroups:
                g = gi % 2
                gi += 1
                n = len(kts)
                base = si
                si += n

                def S_mm(i, base=base, hs=hs, h=h, c0=c0, w=w, kts=kts):
                    s = (base + i) % 3
                    kt = kts[i]
                    P.add("pe", lambda e: e.matmul(ps_s[s][:, 0:w], KTh[hs][:, kt * 128:(kt + 1) * 128], QTs[:, h, c0:c0 + w], start=True, stop=True),
                          reads=[("KTh", hs), "QTs"], writes=[("ps_s", s)])
                for i in range(min(2, n)):
                    S_mm(i)
                for i, kt in enumerate(kts):
                    s = (base + i) % 3
                    P.add("act", lambda e, s=s, w=w: e.activation(pt[s][:, 0:w], ps_s[s][:, 0:w], AF.Exp, scale=scale),
                          reads=[("ps_s", s)], writes=[("ptx", s)])
                    if i + 2 < n:
                        S_mm(i + 2)
                    P.add("pe", lambda e, s=s, g=g, kt=kt, w=w, i=i, n=n, h=h: e.matmul(
                        po[g][0:65, 0:w], Vall[:, kt, h * 65:(h + 1) * 65], pt[s][:, 0:w], start=(i == 0), stop=(i == n - 1)),
                        reads=["Vall", ("ptx", s)], writes=[("po", g)])
                P.add("dve", lambda e, g=g, w=w: e.reciprocal(rden[g][64:65, 0:w], po[g][64:65, 0:w]), reads=[("po", g)], writes=[("rden", g)])
                P.add("pe", lambda e, g=g, w=w: e.matmul(pb[g][0:64, 0:w], ones65[64:65, :], rden[g][64:65, 0:w], start=True, stop=True),
                      reads=["ones65", ("rden", g)], writes=[("pb", g)])
                P.add("act", lambda e, g=g, w=w: e.activation(bcs[g][:, 0:w], pb[g][0:64, 0:w], AF.Copy), reads=[("pb", g)], writes=[("bcs", g)])
                P.add("dve", lambda e, g=g, w=w: e.tensor_tensor(ybt[g][:, 0:w], po[g][0:64, 0:w], bcs[g][:, 0:w], ALU.mult),
                      reads=[("po", g), ("bcs", g)], writes=[("ybt", g)])
                P.add("sp", lambda e, g=g, w=w, c0=c0, h=h: e.dma_start(out=io["mixT"][512 + h * 64:512 + (h + 1) * 64, c0:c0 + w], in_=ybt[g][:, 0:w]),
                      reads=[("ybt", g)], writes=["mixT"], dma=True)
    P.barrier()


def phase_outproj(C, io, layer, wname, mixT, ncols, xl_tiles, ctx_tiles):
    P = C.P
    with ExitStack() as st:
        mix = C.sb(st, "mix", [128, 8, ncols], BF16)
        P.add("sp", lambda e: e.dma_start(out=mix[:], in_=mixT.rearrange("(k p) t -> p k t", p=128)), writes=["mix"], dma=True)
        wo = C.sb(st, "wo", [128, 8, 1024], BF16)
        wsrc = io[wname].rearrange("(k p) d -> p k d", p=128)
        for k in range(0, 8, 2):
            P.add("pool", lambda e, k=k: e.dma_start(out=wo[:, k:k + 2, :], in_=wsrc[:, k:k + 2, :]), writes=["wo"], dma=True)
        g1b = C.sb(st, "g1b", [128, 2, 1024], F32)
        for r in range(2):
            src = io["modrow"][layer, r:r + 1, 2 * 1024:3 * 1024].partition_broadcast(128)
            P.add("sp", lambda e, r=r, src=src: e.dma_start(out=g1b[:, r, :], in_=src), writes=["g1b"], dma=True)
        xt = [C.sb(st, "xt", [128, 1024], F32) for _ in range(2)]
        tmp = [C.sb(st, "tmp", [128, 512], F32) for _ in range(2)]
        po = [C.ps(st, "po", [128, 512]) for _ in range(3)]
        for s_ in range(3):
            C.pskey(("po", s_))
        pi_ = 0
        for i, tix in enumerate(xl_tiles):
            s = i % 2
            r = 1 if tix in ctx_tiles else 0
            P.add("sp", lambda e, s=s, tix=tix: e.dma_start(out=xt[s][:], in_=io["XL"][tix * 128:(tix + 1) * 128, :]),
                  reads=[("XL", tix)], writes=[("xt", s, 0), ("xt", s, 1)], dma=True)
            for hh in range(2):
                p_ = pi_ % 3
                t_ = pi_ % 2
                pi_ += 1
                mm_acc(C, po[p_][:], ("po", p_), [(mix[:, k, i * 128:(i + 1) * 128], wo[:, k, hh * 512:(hh + 1) * 512]) for k in range(8)],
                       ["mix", "wo"])
                P.add("dve", lambda e, p_=p_, t_=t_, r=r, hh=hh: e.tensor_tensor(tmp[t_][:], po[p_][:], g1b[:, r, hh * 512:(hh + 1) * 512], ALU.mult),
                      reads=[("po", p_), "g1b"], writes=[("tmp", t_)])
                P.add("pool", lambda e, s=s, t_=t_, hh=hh: e.tensor_tensor(xt[s][:, hh * 512:(hh + 1) * 512], xt[s][:, hh * 512:(hh + 1) * 512],
                                                                            tmp[t_][:], ALU.add),
                      reads=[("tmp", t_), ("xt", s, hh)], writes=[("xt", s, hh)])
            P.add("sp", lambda e, s=s, tix=tix: e.dma_start(out=io["XL"][tix * 128:(tix + 1) * 128, :], in_=xt[s][:]),
                  reads=[("xt", s, 0), ("xt", s, 1)], writes=[("XL", tix)], dma=True)
    P.barrier()


def phase_qkv1(C, io):
    P = C.P
    with ExitStack() as st:
        hT = C.sb(st, "hT1", [128, 8, NOWN], BF16)
        ident = phase_norm_all(C, io, st, 1, io["XL"], NT_OWN, {0, 1}, hT, False)
        wq = C.sb(st, "wqkv", [128, 8, 1536], BF16)
        wsrc = io["w_qkv"].rearrange("(k p) f -> p k f", p=128)
        for k in range(8):
            P.add("pool", lambda e, k=k: e.dma_start(out=wq[:, k, :], in_=wsrc[:, k, :]), writes=["wqkv"], dma=True)
        qkn = C.sb(st, "qkn", [128, 2, 64], F32)
        for i in range(2):
            P.add("sp", lambda e, i=i: e.dma_start(out=qkn[:, i, :], in_=io["qk_norm"][i:i + 1, :].partition_broadcast(128)), writes=["qkn"], dma=True)
        B2 = lambda nm_, shp, dt: [C.sb(st, nm_, shp, dt) for _ in range(2)]
        cs = B2("cs1", [128, 64], F32)
        sq = B2("sq1", [128, 8, 64], F32)
        ss = B2("ss1", [128, 24], F32)
        rs = B2("rs1", [128, 24], F32)
        kn = B2("kn1", [128, 4, 64], F32)
        kcat = B2("kcat1", [128, 4, 64], F32)
        qn = B2("qn1", [128, 16, 64], F32)
        qcat = B2("qcat1", [128, 16, 64], F32)
        rtmp = B2("rtmp1", [128, 16, 64], F32)
        vaug = B2("vaug1", [128, 4, 65], BF16)
        ktile = B2("ktile1", [64, 4, 128], BF16)
        qtile = B2("qtile1", [64, 16, 128], BF16)
        pq = [C.ps(st, "pq1", [128, 512]) for _ in range(3)]
        pT2 = [C.ps(st, "pT21", [128, 512]) for _ in range(2)]
        for s_ in range(3):
            C.pskey(("pq1", s_)); C.pskey(("pT21", s_))
        for s_ in range(2):
            P.add("pool", lambda e, s_=s_: e.memset(vaug[s_][:], 1.0), writes=[("vaug1", s_)])
        KTd = io["KT1"].rearrange("g f t -> f g t")
        QTd = io["QT1"].rearrange("g f (l j c) -> f g l j c", l=16, j=4)
        ti = 0
        for t in range(NT_OWN):
            s = t % 2
            K = lambda n_, s=s: (n_, s)
            lat = t >= 2
            hasq = 2 <= t < 18
            if lat:
                P.add("sp", lambda e, s=s, t=t: e.dma_start(out=cs[s][:], in_=io["rope_gqa"][(t - 2) * 128:(t - 1) * 128, :]),
                      writes=[K("cs1")], dma=True)
            mm_acc(C, pq[2][:], ("pq1", 2), [(hT[:, k, t * 128:(t + 1) * 128], wq[:, k, 1024:1536]) for k in range(8)], ["hT", "wqkv"])
            kv = pq[2][:].rearrange("p (h c) -> p h c", h=8)
            P.add("act", lambda e, s=s, kv=kv: e.activation(sq[s][:, 0:4, :], kv[:, 0:4, :], AF.Square), reads=[("pq1", 2)], writes=[K("sq1")])
            P.add("dve", lambda e, s=s: e.reduce_sum(ss[s][:, 0:4], sq[s][:, 0:4, :], AX.X), reads=[K("sq1")], writes=[K("ss1")])
            P.add("act", lambda e, s=s, kv=kv: e.activation(vaug[s][:, :, 0:64], kv[:, 4:8, :], AF.Copy), reads=[("pq1", 2)], writes=[K("vaug1")])
            rstd_from_ss(C, rs[s][:, 0:4], ss[s][:, 0:4], 64, K("ss1"), K("rs1"))
            P.add("dve", lambda e, s=s, kv=kv: e.tensor_tensor(kn[s][:], kv[:, 0:4, :], rs[s][:, 0:4].unsqueeze(2).broadcast_to([128, 4, 64]), ALU.mult),
                  reads=[("pq1", 2), K("rs1")], writes=[K("kn1")])
            P.add("pool", lambda e, s=s: e.tensor_tensor(kn[s][:], kn[s][:], qkn[:, 1, :].unsqueeze(1).broadcast_to([128, 4, 64]), ALU.mult),
                  reads=[K("kn1"), "qkn"], writes=[K("kn1")])
            if lat:
                rope_apply(C, kcat[s][:], kn[s][:], cs[s][:], rtmp[s][:, 0:4, :], 4, 32, [K("kn1"), K("cs1")], K("kcat1"), K("rtmp1"))
                ksrc, kkey = kcat[s], K("kcat1")
            else:
                ksrc, kkey = kn[s], K("kn1")
            for g in range(4):
                P.add("pe", lambda e, ksrc=ksrc, g=g: e.transpose(pT2[0][0:64, g * 128:(g + 1) * 128], ksrc[:, g, :], ident[:]),
                      reads=[kkey, "ident"], writes=[("pT21", 0)])
            evac(C, "dve", ktile[s][:], pT2[0][0:64, :].rearrange("p (h c) -> p h c", h=4), [("pT21", 0)], [K("ktile1")])
            P.add("sp", lambda e, s=s, t=t: e.dma_start(out=KTd[:, :, t * 128:(t + 1) * 128], in_=ktile[s][:]), reads=[K("ktile1")], writes=["KT1"], dma=True)
            P.add("sp", lambda e, s=s, t=t: e.dma_start(out=io["V1d"][t * 128:(t + 1) * 128, :], in_=vaug[s][:].rearrange("p h c -> p (h c)")),
                  reads=[K("vaug1")], writes=["V1d"], dma=True)
            if not hasq:
                continue
            for b in range(2):
                mm_acc(C, pq[b][:], ("pq1", b), [(hT[:, k, t * 128:(t + 1) * 128], wq[:, k, b * 512:(b + 1) * 512]) for k in range(8)], ["hT", "wqkv"])
                qv = pq[b][:].rearrange("p (h c) -> p h c", h=8)
                P.add("act", lambda e, s=s, qv=qv: e.activation(sq[s][:], qv, AF.Square), reads=[("pq1", b)], writes=[K("sq1")])
                P.add("dve", lambda e, s=s, b=b: e.reduce_sum(ss[s][:, 8 + 8 * b:16 + 8 * b], sq[s][:], AX.X), reads=[K("sq1")], writes=[K("ss1")])
            rstd_from_ss(C, rs[s][:, 8:24], ss[s][:, 8:24], 64, K("ss1"), K("rs1"))
            for b in range(2):
                qv = pq[b][:].rearrange("p (h c) -> p h c", h=8)
                P.add("dve", lambda e, s=s, b=b, qv=qv: e.tensor_tensor(
                    qn[s][:, 8 * b:8 * b + 8, :], qv, rs[s][:, 8 + 8 * b:16 + 8 * b].unsqueeze(2).broadcast_to([128, 8, 64]), ALU.mult),
                    reads=[("pq1", b), K("rs1")], writes=[K("qn1")])
            P.add("pool", lambda e, s=s: e.tensor_tensor(qn[s][:], qn[s][:], qkn[:, 0, :].unsqueeze(1).broadcast_to([128, 16, 64]), ALU.mult),
                  reads=[K("qn1"), "qkn"], writes=[K("qn1")])
            rope_apply(C, qcat[s][:], qn[s][:], cs[s][:], rtmp[s][:], 16, 32, [K("qn1"), K("cs1")], K("qcat1"), K("rtmp1"))
            for g in range(4):
                b = g % 2
                for j in range(4):
                    P.add("pe", lambda e, s=s, b=b, g=g, j=j: e.transpose(pT2[b][0:64, j * 128:(j + 1) * 128], qcat[s][:, g * 4 + j, :], ident[:]),
                          reads=[K("qcat1"), "ident"], writes=[("pT21", b)])
                evac(C, "act" if b else "dve", qtile[s][:, g * 4:(g + 1) * 4, :], pT2[b][0:64, :].rearrange("p (h c) -> p h c", h=4),
                     [("pT21", b)], [K("qtile1")])
            lt = t - 2
            for g in range(4):
                P.add("sp", lambda e, s=s, lt=lt, g=g: e.dma_start(out=QTd[:, g, lt, :, :], in_=qtile[s][:, g * 4:(g + 1) * 4, :]),
                      reads=[K("qtile1")], writes=["QT1"], dma=True)
    P.barrier()


def phase_attn1(C, io):
    P = C.P
    scale = 64.0 ** -0.5
    with ExitStack() as st:
        KTs = C.sb(st, "KT1s", [64, 4, NOWN], BF16)
        P.add("sp", lambda e: e.dma_start(out=KTs[:], in_=io["KT1"].rearrange("g f t -> f g t")), writes=["KT1s"], dma=True)
        V1 = C.sb(st, "V1s", [128, NT_OWN, 260], BF16)
        P.add("sp", lambda e: e.dma_start(out=V1[:], in_=io["V1d"].rearrange("(t p) c -> p t c", p=128)), writes=["V1s"], dma=True)
        QTs = C.sb(st, "QT1s", [64, 4, 16 * 512], BF16)
        for g in range(4):
            P.add("sp", lambda e, g=g: e.dma_start(out=QTs[:, g, :], in_=io["QT1"][g]), writes=["QT1s"], dma=True)
        msk = C.sb(st, "msk", [128, 2, 128], F32)
        P.add("sp", lambda e: e.dma_start(out=msk[:], in_=io["masks"].rearrange("m k q -> k m q")), writes=["msk"], dma=True)
        snk = C.sb(st, "snk", [65, 4, 512], F32)
        P.add("sp", lambda e: e.dma_start(out=snk[64:65, :, :], in_=io["sink_rep"].rearrange("(o g) c -> o g c", o=1)), writes=["snk"], dma=True)
        P.add("act", lambda e: e.activation(snk[64:65, :, :], snk[64:65, :, :], AF.Exp), reads=["snk"], writes=["snk"])
        ones65 = C.sb(st, "ones65", [65, 64], F32)
        P.add("pool", lambda e: e.memset(ones65[:], 1.0), writes=["ones65"])
        pt = [C.sb(st, "ptx", [128, 512], BF16) for _ in range(3)]
        rden = [C.sb(st, "rden", [65, 512], F32) for _ in range(2)]
        bcs = [C.sb(st, "bcs", [64, 512], F32) for _ in range(2)]
        ybt = [C.sb(st, "ybt", [64, 512], BF16) for _ in range(2)]
        ps_s = [C.ps(st, "ps_s", [128, 512]) for _ in range(3)]
        po = [C.ps(st, "po", [128, 512]) for _ in range(2)]
        pb = [C.ps(st, "pb", [128, 512]) for _ in range(2)]
        for s_ in range(3):
            C.pskey(("ps_s", s_)); C.pskey(("po", s_)); C.pskey(("pb", s_))
        Md = io["mixT1"].rearrange("(h f) t -> f h t", f=64)
        si = 0
        gi = 0
        for lt in range(16):
            kts = ([(lt + 1, 0)] if lt >= 1 else []) + [(lt + 2, None), (lt + 3, 1), (0, None), (1, None)]
            for g in range(4):
                gg = gi % 2
                gi += 1
                n = len(kts)
                base = si
                si += n

                def S_mm(i, base=base, g=g, lt=lt, kts=kts):
                    s = (base + i) % 3
                    kt = kts[i][0]
                    P.add("pe", lambda e: e.matmul(ps_s[s][:], KTs[:, g, kt * 128:(kt + 1) * 128], QTs[:, g, lt * 512:(lt + 1) * 512], start=True, stop=True),
                          reads=["KT1s", "QT1s"], writes=[("ps_s", s)])
                for i in range(min(2, n)):
                    S_mm(i)
                for i, (kt, m) in enumerate(kts):
                    s = (base + i) % 3
                    P.add("act", lambda e, s=s: e.activation(pt[s][:], ps_s[s][:], AF.Exp, scale=scale), reads=[("ps_s", s)], writes=[("ptx", s)])
                    if m is not None:
                        P.add("dve", lambda e, s=s, m=m: e.tensor_tensor(
                            pt[s][:].rearrange("p (j c) -> p j c", j=4), pt[s][:].rearrange("p (j c) -> p j c", j=4),
                            msk[:, m, :].unsqueeze(1).broadcast_to([128, 4, 128]), ALU.mult),
                            reads=[("ptx", s), "msk"], writes=[("ptx", s)])
                    if i + 2 < n:
                        S_mm(i + 2)
                    P.add("pe", lambda e, s=s, gg=gg, kt=kt, g=g, i=i, n=n: e.matmul(
                        po[gg][0:65, :], V1[:, kt, g * 65:(g + 1) * 65], pt[s][:], start=(i == 0), stop=(i == n - 1)),
                        reads=["V1s", ("ptx", s)], writes=[("po", gg)])
                P.add("dve", lambda e, gg=gg, g=g: e.tensor_tensor(rden[gg][64:65, :], po[gg][64:65, :], snk[64:65, g, :], ALU.add),
                      reads=[("po", gg), "snk"], writes=[("rden", gg)])
                P.add("dve", lambda e, gg=gg: e.reciprocal(rden[gg][64:65, :], rden[gg][64:65, :]), reads=[("rden", gg)], writes=[("rden", gg)])
                P.add("pe", lambda e, gg=gg: e.matmul(pb[gg][0:64, :], ones65[64:65, :], rden[gg][64:65, :], start=True, stop=True),
                      reads=["ones65", ("rden", gg)], writes=[("pb", gg)])
                P.add("act", lambda e, gg=gg: e.activation(bcs[gg][:], pb[gg][0:64, :], AF.Copy), reads=[("pb", gg)], writes=[("bcs", gg)])
                P.add("dve", lambda e, gg=gg: e.tensor_tensor(ybt[gg][:], po[gg][0:64, :], bcs[gg][:], ALU.mult),
                      reads=[("po", gg), ("bcs", gg)], writes=[("ybt", gg)])
                P.add("sp", lambda e, gg=gg, g=g, lt=lt: e.dma_start(out=Md[:, g * 4:(g + 1) * 4, lt * 128:(lt + 1) * 128],
                                                                    in_=ybt[gg][:].rearrange("p (j c) -> p j c", j=4)),
                      reads=[("ybt", gg)], writes=["mixT1"], dma=True)
    P.barrier()


IN_SPECS = {
    "xall": ([NALL, D], F32),
    "cT": ([128, 16], F32),
    "w_mod": ([2, D, 6 * D], F32),
    "b_mod": ([2, 6 * D], F32),
    "normT": ([128, 32], F32),
    "ident": ([128, 128], F32),
    "w_in": ([D, 1440], F32),
    "lruvec": ([128, 64], F32),
    "wbd": ([16, 128, 128], F32),
    "q_a_normT": ([128, 2], F32),
    "kv_a_normT": ([128, 1], F32),
    "w_q_b": ([256, 768], F32),
    "w_kv_b": ([128, 1024], F32),
    "nope_norm": ([2, 64], F32),
    "rope_norm": ([2, 32], F32),
    "rope_mla": ([SEQ, 32], F32),
    "w_out_even": ([D, D], F32),
    "w_qkv": ([D, 1536], F32),
    "qk_norm": ([2, 64], F32),
    "sink_rep": ([4, 512], F32),
    "w_out_odd": ([D, D], F32),
    "rope_gqa": ([2176, 64], F32),
    "masks": ([2, 128, 128], F32),
    "w_router": ([2, D, NE], F32),
    "b_router": ([2, NE], F32),
    "w_guR": ([2 * NE * 8, 128, 2048], F32),
    "b_guT": ([2, 128, NE * 16], F32),
    "w_down": ([2 * NE, D, D], F32),
    "b_down": ([2 * NE, D], F32),
}


def build_program(cfg):
    nc = bass.Bass("TRN2", target_bir_lowering=False)
    io = {}
    used = cfg.get("inputs", list(IN_SPECS))
    nea = cfg.get("ne_alloc", NE)
    for n in used:
        shp, dt = IN_SPECS[n]
        shp = [2 * nea if (v == 2 * NE and n in ("w_down", "b_down")) else (2 * nea * 8 if (v == 2 * NE * 8 and n == "w_guR") else v) for v in shp]
        io[n] = nc.dram_tensor(n, shp, dt, kind="ExternalInput").ap()
    io["nea"] = nea
    io["modrow"] = nc.dram_tensor("modrow", [2, 2, 6 * D], F32, kind="Internal").ap()
    io["XL"] = nc.dram_tensor("XL", [NOWN, D], F32, kind="Internal").ap()
    io["xaT"] = nc.dram_tensor("xaT", [512, NALL], F32, kind="Internal").ap()
    io["gaT"] = nc.dram_tensor("gaT", [512, NOWN], F32, kind="Internal").ap()
    io["qkr"] = nc.dram_tensor("qkr", [NALL, 416], F32, kind="Internal").ap()
    io["mixT"] = nc.dram_tensor("mixT", [D, NOWN], BF16, kind="Internal").ap()
    io["QT"] = nc.dram_tensor("QT", [8, 96, NOWN], BF16, kind="Internal").ap()
    io["KT"] = nc.dram_tensor("KT", [8, 96, NALL], BF16, kind="Internal").ap()
    io["Vd"] = nc.dram_tensor("Vd", [NALL, 520], BF16, kind="Internal").ap()
    io["mixT1"] = nc.dram_tensor("mixT1", [D, 2048], BF16, kind="Internal").ap()
    io["QT1"] = nc.dram_tensor("QT1", [4, 64, 16 * 512], BF16, kind="Internal").ap()
    io["KT1"] = nc.dram_tensor("KT1", [4, 64, NOWN], BF16, kind="Internal").ap()
    io["V1d"] = nc.dram_tensor("V1d", [NOWN, 260], BF16, kind="Internal").ap()
    io["out"] = nc.dram_tensor("out", [2048, D], F32, kind="ExternalOutput").ap()
    if cfg.get("dbg_xlin"):
        io["XLin"] = nc.dram_tensor("XLin", [NOWN, D], F32, kind="ExternalInput").ap()
    if cfg.get("dbg_xlout"):
        io["XLout"] = nc.dram_tensor("XLout", [NOWN, D], F32, kind="ExternalOutput").ap()
    if cfg.get("dbg_mod"):
        io["modout"] = nc.dram_tensor("modout", [2, 2, 6 * D], F32, kind="ExternalOutput").ap()
    with ExitStack() as stack:
        P = Prog(nc, stack)
        C = Ctx(nc, P)
        C.ne = cfg.get("ne", NE)
        C.lvl = cfg.get("lvl", 9)
        C.nowdma = cfg.get("nowdma", 0)
        phases = cfg["phases"]
        if cfg.get("dbg_xlin"):
            for t in range(NT_OWN):
                P.add("sp", lambda e, t=t: e.dma_start(out=io["XL"][t * 128:(t + 1) * 128, :], in_=io["XLin"][t * 128:(t + 1) * 128, :]),
                      writes=["XL"], dma=True)
            P.barrier()
        if "mod" in phases:
            phase_mod(C, io)
        if cfg.get("dbg_mod"):
            P.add("sp", lambda e: e.dma_start(out=io["modout"], in_=io["modrow"]), reads=["modrow"], writes=["modout"], dma=True)
        if "mix0" in phases:
            phase_inproj0(C, io)
            phase_lru(C, io)
            phase_mla_prep(C, io)
            phase_mla_attn(C, io)
            phase_outproj(C, io, 0, "w_out_even", io["mixT"], NOWN, list(range(NT_OWN)), {0, 1})
        if "moe0" in phases:
            t0 = cfg.get("moe0_tiles", [list(range(0, 10)), list(range(10, 19))])
            for tl in t0:
                phase_moe(C, io, 0, tl, {0, 1}, io["XL"])
        if "mix1" in phases:
            phase_qkv1(C, io)
            phase_attn1(C, io)
            phase_outproj(C, io, 1, "w_out_odd", io["mixT1"], 2048, list(range(2, 18)), set())
        if "moe1" in phases:
            t1 = cfg.get("moe1_tiles", [list(range(2, 10)), list(range(10, 18))])
            for tl in t1:
                phase_moe(C, io, 1, tl, set(), io["XL"], out_ap=io["out"], out_tiles={t: t - 2 for t in range(2, 18)})
        if cfg.get("dbg_xlout"):
            P.barrier()
            for t in range(NT_OWN):
                P.add("sp", lambda e, t=t: e.dma_start(out=io["XLout"][t * 128:(t + 1) * 128, :], in_=io["XL"][t * 128:(t + 1) * 128, :]),
                      reads=["XL"], writes=["XLout"], dma=True)
        P.barrier()
        P.add("sp", None)
        P.emit()
        cfg["n_ops"] = P.n_ops
    return nc


def local_rows(half):
    lat = np.arange(SEQ) if half == 0 else np.arange(SEQ)[::-1]
    cx = np.arange(NCTX) if half == 0 else np.arange(NCTX)[::-1]
    return lat, cx


def rope_table(pos, rot_dim):
    pos = np.asarray(pos)
    row = (pos // 64).astype(np.float32)
    col = (pos % 64).astype(np.float32)
    n = rot_dim // 4
    freqs = (np.float32(10000.0) ** (-np.arange(n, dtype=np.float32) / np.float32(n))).astype(np.float32)
    ang = np.concatenate([row[:, None] * freqs, col[:, None] * freqs], axis=-1).astype(np.float32)
    return np.ascontiguousarray(np.concatenate([np.cos(ang), np.sin(ang)], axis=-1).astype(np.float32))


def prep_inputs(inputs, used=None):
    f = lambda a: np.ascontiguousarray(np.asarray(a, dtype=np.float32))
    x, c, ctx, c_ctx = f(inputs["x"]), f(inputs["c"]), f(inputs["ctx"]), f(inputs["c_ctx"])
    shared = {}
    shared["w_mod"] = f(inputs["w_mod"])
    shared["b_mod"] = f(inputs["b_mod"])
    nv = np.stack([inputs["norm_mix"][0], inputs["norm_ffn"][0], inputs["norm_mix"][1], inputs["norm_ffn"][1]])
    shared["normT"] = f(np.asarray(nv).reshape(4, 8, 128).transpose(2, 0, 1).reshape(128, 32))
    shared["ident"] = np.eye(128, dtype=np.float32)
    shared["w_router"] = f(inputs["w_router"])
    shared["b_router"] = f(inputs["b_router"])
    wg = np.asarray(inputs["w_gate_up"], dtype=np.float32).reshape(2, NE, 8, 128, 2, 8, 128)
    shared["w_guR"] = np.ascontiguousarray(wg.transpose(0, 1, 5, 3, 4, 2, 6)).reshape(2 * NE * 8, 128, 2048)
    shared["b_guT"] = f(np.asarray(inputs["b_gate_up"]).reshape(2, NE, 16, 128).transpose(0, 3, 1, 2).reshape(2, 128, NE * 16))
    shared["w_down"] = f(inputs["w_down"]).reshape(2 * NE, D, D)
    shared["b_down"] = f(inputs["b_down"]).reshape(2 * NE, D)
    shared["w_in"] = f(inputs["w_in_even"][0])
    shared["q_a_normT"] = f(np.asarray(inputs["mla_q_a_norm"][0]).reshape(2, 128).T)
    shared["kv_a_normT"] = f(np.asarray(inputs["mla_kv_a_norm"][0]).reshape(1, 128).T)
    shared["w_q_b"] = f(inputs["mla_w_q_b"][0])
    shared["w_kv_b"] = f(inputs["mla_w_kv_b"][0])
    shared["nope_norm"] = f(inputs["mla_nope_norm"][0])
    shared["rope_norm"] = f(inputs["mla_rope_norm"][0])
    shared["w_out_even"] = f(inputs["w_out_even"][0])
    shared["w_qkv"] = f(inputs["w_qkv_odd"][0])
    shared["qk_norm"] = f(inputs["gqa_qk_norm"][0])
    shared["sink_rep"] = f(np.repeat(np.asarray(inputs["gqa_sink"][0]).reshape(4, 4, 1), 128, axis=2).reshape(4, 512))
    shared["w_out_odd"] = f(inputs["w_out_odd"][0])
    kk_, qq_ = np.meshgrid(np.arange(128), np.arange(128), indexing="ij")
    shared["masks"] = f(np.stack([(kk_ >= qq_), (kk_ <= qq_)]).astype(np.float32))
    lru = {k: np.asarray(inputs[k][0], dtype=np.float32) for k in
           ("lru_conv_w", "lru_conv_b", "lru_w_r", "lru_b_r", "lru_w_i", "lru_b_i", "lru_lambda")}
    maps = []
    for core in range(8):
        b, half = core // 2, core % 2
        lat, cx = local_rows(half)
        m = dict(shared)
        m["xall"] = f(np.concatenate([ctx[b][cx], x[b][lat]], axis=0))
        vecs = np.stack([c[b], c_ctx])
        m["cT"] = f(vecs.reshape(2, 8, 128).transpose(2, 1, 0).reshape(128, 16))
        lv = np.zeros((128, 2, 4, 8), np.float32)
        wbd = np.zeros((2, 2, 4, 128, 128), np.float32)
        for dl in range(2):
            d = dl if half == 0 else 1 - dl
            for kc in range(4):
                ch = slice(kc * 128, (kc + 1) * 128)
                for j in range(4):
                    lv[:, dl, kc, j] = lru["lru_conv_w"][d, j, ch]
                lv[:, dl, kc, 4] = lru["lru_conv_b"][d, ch]
                lv[:, dl, kc, 5] = lru["lru_b_r"][d, ch]
                lv[:, dl, kc, 6] = lru["lru_b_i"][d, ch]
                lv[:, dl, kc, 7] = lru["lru_lambda"][d, ch]
                for g_, wn in enumerate(("lru_w_r", "lru_w_i")):
                    for bb in range(2):
                        wbd[g_, dl, kc, bb * 64:(bb + 1) * 64, bb * 64:(bb + 1) * 64] = lru[wn][d, kc * 2 + bb]
        m["lruvec"] = f(lv.reshape(128, 64))
        m["wbd"] = f(wbd.reshape(16, 128, 128))
        m["rope_mla"] = rope_table(lat, 32)
        m["rope_gqa"] = rope_table(lat[:2176], 64)
        if used is not None:
            m = {k: v for k, v in m.items() if k in used}
        maps.append(m)
    return maps


def kernel(**inputs):
    cfg = {"phases": ["mod", "mix0", "moe0", "mix1", "moe1"]}
    nc = build_program(cfg)
    maps = prep_inputs(inputs)
    res = run_bass_kernel_spmd(nc, maps, core_ids=list(range(8)))
    out = np.zeros((4, SEQ, D), np.float32)
    for core in range(8):
        b, half = divmod(core, 2)
        lat, _ = local_rows(half)
        out[b, lat[:2048]] = np.asarray(res.results[core]["out"], dtype=np.float32)
    return out
```

```python
import math
from contextlib import ExitStack

import numpy as np
import concourse.bass as bass
import concourse.mybir as mybir
from concourse.bass_utils import run_bass_kernel_spmd

F32 = mybir.dt.float32
BF16 = mybir.dt.bfloat16
AF = mybir.ActivationFunctionType
ALU = mybir.AluOpType
AX = mybir.AxisListType

D = 1024
NCTX = 256
SEQ = 4096
NALL = NCTX + SEQ
NOWN = NCTX + 2048 + 128
NT_ALL = NALL // 128
NT_OWN = NOWN // 128
NE = 32
EPS = 1e-6


class Op:
    __slots__ = ("eng", "fn", "is_dma", "deps", "needs_inc", "ticket", "sem", "semval", "idx")


class Prog:
    CENG = ("pe", "act", "dve", "pool")

    def __init__(self, nc, stack):
        self.nc = nc
        self.ops = []
        self.esem = {e: stack.enter_context(nc.semaphore("s_" + e)) for e in self.CENG}
        self.dpool = {}
        for q, n in (("sp", 16), ("act", 8), ("pool", 16)):
            self.dpool[q] = [stack.enter_context(nc.semaphore("d_%s%d" % (q, i))) for i in range(n)]
        self.dcount = {q: 0 for q in self.dpool}
        self.last_dma = {}
        self.lastw = {}
        self.readers = {}
        self.last_on_eng = {}
        self.pending_bar = {}
        self.pskeys = set()
        self.defer = None

    def interleave(self, bodies, width=2):
        if self.defer is not None or width <= 1:
            for b in bodies:
                b()
            return
        lists = []
        for b in bodies:
            self.defer = []
            b()
            lists.append(self.defer)
            self.defer = None
        active = []
        nxt = 0
        while nxt < len(lists) or active:
            while len(active) < width and nxt < len(lists):
                active.append([lists[nxt], 0])
                nxt += 1
            for a in list(active):
                lst = a[0]
                while a[1] < len(lst):
                    args, glue = lst[a[1]]
                    self.add(*args)
                    a[1] += 1
                    if not glue:
                        break
                if a[1] >= len(lst):
                    active.remove(a)

    def add(self, eng, fn, reads=(), writes=(), dma=False, glue=False):
        if self.defer is not None:
            self.defer.append(((eng, fn, tuple(reads), tuple(writes), dma), glue))
            return None
        xs = [r for r in reads if r in self.pskeys and r not in writes]
        if xs:
            writes = list(writes) + xs
        op = Op()
        op.eng, op.fn, op.is_dma = eng, fn, dma
        op.needs_inc, op.ticket, op.sem, op.semval = False, 0, None, 0
        op.idx = len(self.ops)
        deps = {}

        def dep(o, kind):
            if o is None or o is op:
                return
            if deps.get(o) != "raw":
                deps[o] = kind

        for r in reads:
            dep(self.lastw.get(r), "raw")
        for w in writes:
            dep(self.lastw.get(w), "w")
            for o in self.readers.get(w, ()):
                dep(o, "w")
        for o in self.pending_bar.pop(eng, ()):
            dep(o, "w")
        if dma:
            q = eng
            j = self.dcount[q]
            K = len(self.dpool[q])
            k = j % K
            dep(self.last_dma.get((q, k)), "raw")
            op.sem = self.dpool[q][k]
            op.semval = 16 * (j // K + 1)
            self.last_dma[(q, k)] = op
            self.dcount[q] += 1
            op.needs_inc = True
        else:
            op.sem = self.esem.get(eng)
        for r in reads:
            self.readers.setdefault(r, []).append(op)
        for w in writes:
            self.lastw[w] = op
            self.readers[w] = []
        kept = []
        for o, kind in deps.items():
            if (not o.is_dma) and (not op.is_dma) and o.eng == eng:
                if eng == "pe":
                    continue
            if not o.is_dma:
                o.needs_inc = True
            kept.append(o)
        op.deps = kept
        self.ops.append(op)
        if not dma:
            self.last_on_eng[eng] = op
        return op

    def barrier(self):
        outstanding = list(self.last_on_eng.values()) + list(self.last_dma.values())
        for e in ("pe", "act", "dve", "pool", "sp"):
            self.pending_bar[e] = list(self.pending_bar.get(e, ())) + outstanding
        self.lastw = {}
        self.readers = {}

    def emit(self):
        nc = self.nc
        cnt = {e: 0 for e in self.CENG}
        for op in self.ops:
            if not op.is_dma and op.needs_inc:
                cnt[op.eng] += 1
                op.semval = cnt[op.eng]
        by_eng = {e: [] for e in ("pe", "act", "dve", "pool", "sp")}
        for op in self.ops:
            by_eng[op.eng].append(op)

        def runner(name):
            def body(eng):
                known = {}
                for op in by_eng[name]:
                    for o in op.deps:
                        if known.get(o.sem, 0) >= o.semval:
                            continue
                        eng.wait_ge(o.sem, o.semval)
                        known[o.sem] = o.semval
                    if op.fn is None:
                        continue
                    ins = op.fn(eng)
                    if op.needs_inc:
                        ins.then_inc(op.sem, 16 if op.is_dma else 1)
            return body

        with nc.Block() as block:
            block.tensor(runner("pe"))
            block.scalar(runner("act"))
            block.vector(runner("dve"))
            block.gpsimd(runner("pool"))
            block.sync(runner("sp"))
        self.n_ops = {e: len(v) for e, v in by_eng.items()}


class Ctx:
    def __init__(self, nc, P):
        self.nc = nc
        self.P = P
        self.uid = 0

    def name(self, base):
        self.uid += 1
        return "%s_%d" % (base, self.uid)

    def sb(self, stack, base, shape, dtype):
        n = self.name(base)
        return stack.enter_context(self.nc.sbuf_tensor(n, list(shape), dtype))

    def ps(self, stack, base, shape, dtype=F32):
        n = self.name(base)
        t = stack.enter_context(self.nc.psum_tensor(n, [128, 512], F32))
        return t

    def pskey(self, key):
        self.P.pskeys.add(key)
        return key


def tile_groups(ntiles, maxt=4):
    out = []
    t = 0
    while t < ntiles:
        n = min(maxt, ntiles - t)
        out.append((t, n))
        t += n
    return out


def phase_mod(C, io):
    nc, P = C.nc, C.P
    with ExitStack() as st:
        cT = C.sb(st, "cT", [128, 16], F32)
        sg = C.sb(st, "csg", [128, 16], F32)
        cs = C.sb(st, "cs", [128, 16], F32)
        ones2 = C.sb(st, "ones2", [1, 2], F32)
        brow = [C.sb(st, "brow", [1, 512], F32) for _ in range(2)]
        wm = [C.sb(st, "wm", [128, 8, 512], F32) for _ in range(2)]
        orow = [C.sb(st, "orow", [2, 512], F32) for _ in range(2)]
        pm = [C.ps(st, "pm", [2, 512]) for _ in range(2)]
        for s_ in range(2):
            C.pskey(("pm", s_))
        P.add("sp", lambda e: e.dma_start(out=cT[:], in_=io["cT"]), writes=["cT"], dma=True)
        P.add("pool", lambda e: e.memset(ones2[:], 1.0), writes=["ones2"])
        P.add("act", lambda e: e.activation(sg[:], cT[:], AF.Sigmoid), reads=["cT"], writes=["csg"])
        P.add("dve", lambda e: e.tensor_tensor(cs[:], cT[:], sg[:], ALU.mult), reads=["cT", "csg"], writes=["cs"])
        it = 0
        for layer in range(2):
            for g in range(12):
                s = it % 2
                it += 1
                src = io["w_mod"][layer].rearrange("(k p) f -> p k f", p=128)[:, :, g * 512:(g + 1) * 512]
                P.add("sp", lambda e, s=s, src=src: e.dma_start(out=wm[s][:], in_=src),
                      writes=[("wm", s)], dma=True)
                bsrc = io["b_mod"][layer:layer + 1, g * 512:(g + 1) * 512]
                P.add("sp", lambda e, s=s, bsrc=bsrc: e.dma_start(out=brow[s][:], in_=bsrc),
                      writes=[("brow", s)], dma=True)
                for k in range(8):
                    P.add("pe", lambda e, s=s, k=k: e.matmul(pm[s][0:2, :], cs[:, 2 * k:2 * k + 2], wm[s][:, k, :],
                                                             start=(k == 0), stop=False),
                          reads=["cs", ("wm", s)], writes=[("pm", s)])
                P.add("pe", lambda e, s=s: e.matmul(pm[s][0:2, :], ones2[:], brow[s][:], start=False, stop=True),
                      reads=["ones2", ("brow", s)], writes=[("pm", s)])
                P.add("dve", lambda e, s=s: e.tensor_copy(orow[s][:], pm[s][0:2, :]), reads=[("pm", s)],
                      writes=[("orow", s)])
                dst = io["modrow"][layer, :, g * 512:(g + 1) * 512]
                P.add("sp", lambda e, s=s, dst=dst: e.dma_start(out=dst, in_=orow[s][:]),
                      reads=[("orow", s)], writes=["modrow"], dma=True)
    P.barrier()


class NormBufs:
    def __init__(self, C, st, tag):
        self.tag = tag
        self.junk = C.sb(st, "junk", [128, 1024], F32)
        self.xn = [C.sb(st, "xn", [128, 1024], F32) for _ in range(2)]
        self.ss = [C.sb(st, "ss", [128, 1], F32) for _ in range(2)]
        self.rs = [C.sb(st, "rs", [128, 1], F32) for _ in range(2)]
        self.pt = [[C.ps(st, "pt", [128, 4, 128]) for _ in range(2)] for _ in range(2)]
        for s_ in range(2):
            for hh in range(2):
                C.pskey((tag, "pt", s_, hh))
        self.i = 0


def norm_tile(C, NB, ident, xsrc, xkey, gm, sh, vec_keys, dst_bf, dst_key, dst_f32=None, dst32_key=None, s=None):
    P = C.P
    if s is None:
        s = NB.i % 2
    NB.i += 1
    t = NB.tag
    junk, xn, ss, rs = NB.junk, NB.xn[s], NB.ss[s], NB.rs[s]
    P.add("act", lambda e: e.activation(junk[:], xsrc, AF.Square, accum_out=ss[:]),
          reads=[xkey], writes=[(t, "junk"), (t, "ss", s)])
    P.add("act", lambda e: e.activation(rs[:], ss[:], AF.Sqrt, bias=EPS, scale=1.0 / D),
          reads=[(t, "ss", s)], writes=[(t, "rs", s)])
    P.add("dve", lambda e: e.reciprocal(rs[:], rs[:]), reads=[(t, "rs", s)], writes=[(t, "rs", s)])
    P.add("dve", lambda e: e.tensor_scalar(xn[:], xsrc, rs[:, 0:1], None, ALU.mult),
          reads=[xkey, (t, "rs", s)], writes=[(t, "xn", s)])
    for hh in range(2):
        pt = NB.pt[s][hh]
        for kk in range(4):
            k = hh * 4 + kk
            P.add("pe", lambda e, k=k, kk=kk, pt=pt: e.transpose(pt[:, kk * 128:(kk + 1) * 128], xn[:, k * 128:(k + 1) * 128], ident[:]),
                  reads=[(t, "xn", s), "ident"], writes=[(t, "pt", s, hh)])
        for kk in range(4):
            k = hh * 4 + kk
            if dst_f32 is not None:
                P.add("dve", lambda e, k=k, kk=kk, pt=pt: e.tensor_scalar(dst_f32(k), pt[:, kk * 128:(kk + 1) * 128], gm[:, k:k + 1],
                                                                       sh[:, k:k + 1], ALU.mult, ALU.add),
                      reads=[(t, "pt", s, hh)] + list(vec_keys), writes=[dst32_key])
                P.add("act", lambda e, k=k: e.activation(dst_bf(k), dst_f32(k), AF.Copy),
                      reads=[dst32_key], writes=[dst_key])
            else:
                P.add("act", lambda e, k=k, kk=kk, pt=pt: e.activation(dst_bf(k), pt[:, kk * 128:(kk + 1) * 128], AF.Identity,
                                                                    bias=sh[:, k:k + 1], scale=gm[:, k:k + 1]),
                      reads=[(t, "pt", s, hh)] + list(vec_keys), writes=[dst_key])


def load_modcols(C, st, io, layer, which):
    P = C.P
    t = C.sb(st, "mc", [128, 2, 8], F32)
    key = C.name("mck")
    for r in range(2):
        src = io["modrow"][layer, r, which * 1024:(which + 1) * 1024].rearrange("(k p) -> p k", p=128)
        P.add("sp", lambda e, r=r, src=src: e.dma_start(out=t[:, r, :], in_=src, allow_slow_non_contiguous=True),
              reads=["modrow"], writes=[key], dma=True)
    return t, key


def phase_moe(C, io, layer, tiles, ctx_tiles, XL, out_ap=None, out_tiles=None):
    nc, P = C.nc, C.P
    NTL = len(tiles)
    NTOK = NTL * 128
    groups = tile_groups(NTL, 4)
    with ExitStack() as st:
        XS = C.sb(st, "XS", [128, NTL, 1024], F32)
        fT = C.sb(st, "fT", [128, 8, NTOK], BF16)
        gates = C.sb(st, "gates", [128, NTL, NE], F32)
        ident = C.sb(st, "ident", [128, 128], F32)
        P.add("sp", lambda e: e.dma_start(out=ident[:], in_=io["ident"]), writes=["ident"], dma=True)
        with ExitStack() as st2:
            NB = NormBufs(C, st2, "moenb")
            sc2, k1 = load_modcols(C, st2, io, layer, 4)
            sh2, k2 = load_modcols(C, st2, io, layer, 3)
            nf = C.sb(st2, "nf", [128, 8], F32)
            gm = C.sb(st2, "gm", [128, 2, 8], F32)
            P.add("sp", lambda e: e.dma_start(out=nf[:], in_=io["normT"][:, (2 * layer + 1) * 8:(2 * layer + 2) * 8]),
                  writes=["nf"], dma=True)
            for r in range(2):
                if C.lvl == 0:
                    continue
                P.add("dve", lambda e, r=r: e.scalar_tensor_tensor(gm[:, r, :], sc2[:, r, :], 1.0, nf[:], ALU.add, ALU.mult),
                      reads=[k1, "nf"], writes=["gmv"])
            wr = C.sb(st2, "wr", [128, 8, NE], F32)
            P.add("sp", lambda e: e.dma_start(out=wr[:], in_=io["w_router"][layer].rearrange("(k p) n -> p k n", p=128)),
                  writes=["wr"], dma=True)
            brb = C.sb(st2, "brb", [128, NE], F32)
            P.add("sp", lambda e: e.dma_start(out=brb[:], in_=io["b_router"][layer:layer + 1, :].partition_broadcast(128)),
                  writes=["brb"], dma=True)
            f32t = [C.sb(st2, "f32t", [128, 8, 128], F32) for _ in range(2)]
            pl = [C.ps(st2, "pl", [128, NE]) for _ in range(2)]
            for s_ in range(2):
                C.pskey(("pl", s_))
            lg = [C.sb(st2, "lg", [128, NE], F32) for _ in range(2)]
            m8 = [C.sb(st2, "m8", [128, 8], F32) for _ in range(2)]
            ex = [C.sb(st2, "ex", [128, NE], F32) for _ in range(2)]
            mk = [C.sb(st2, "mk", [128, NE], F32) for _ in range(2)]
            sm = [C.sb(st2, "sm", [128, 1], F32) for _ in range(2)]
            nm = [C.sb(st2, "nm", [128, 1], F32) for _ in range(2)]
            bdall = C.sb(st2, "bdall", [NE, 1024], F32)
            nea_ = io["nea"]
            if C.ne == NE:
                P.add("sp", lambda e: e.dma_start(out=bdall[:], in_=io["b_down"][layer * nea_:layer * nea_ + NE, :]), writes=["bdall"], dma=True)
            g2p = C.sb(st2, "g2p", [128, 2, 1024], F32)
            for r in range(2):
                src = io["modrow"][layer, r:r + 1, 5 * 1024:6 * 1024].partition_broadcast(128)
                P.add("sp", lambda e, r=r, src=src: e.dma_start(out=g2p[:, r, :], in_=src), reads=["modrow"], writes=["g2p"], dma=True)
            gT = [C.sb(st2, "gT", [NE, 128], F32) for _ in range(2)]
            btmp = [C.sb(st2, "btmp", [128, 512], F32) for _ in range(2)]
            pbias = [C.ps(st2, "pbias", [128, 512]) for _ in range(2)]
            for s_ in range(2):
                C.pskey(("pbias", s_))
            def pbody(i, tix):
                s = i % 2
                r = 1 if tix in ctx_tiles else 0
                P.add("sp", lambda e, i=i, tix=tix: e.dma_start(out=XS[:, i, :], in_=XL[tix * 128:(tix + 1) * 128, :]),
                      reads=["XL"], writes=[("XS", i)], dma=True)
                norm_tile(C, NB, ident, XS[:, i, :], ("XS", i), gm[:, r, :], sh2[:, r, :], ["gmv", k2],
                          lambda k, i=i: fT[:, k, i * 128:(i + 1) * 128], "fT",
                          dst_f32=lambda k, s=s: f32t[s][:, k, :], dst32_key=("f32t", s), s=s)
                for k in range(8):
                    P.add("pe", lambda e, s=s, k=k: e.matmul(pl[s][:, 0:NE], f32t[s][:, k, :], wr[:, k, :],
                                                             start=(k == 0), stop=(k == 7)),
                          reads=[("f32t", s), "wr"], writes=[("pl", s)])
                P.add("dve", lambda e, s=s: e.tensor_tensor(lg[s][:], pl[s][:, 0:NE], brb[:], ALU.add),
                      reads=[("pl", s), "brb"], writes=[("lg", s)])
                P.add("dve", lambda e, s=s: e.max(m8[s][:], lg[s][:]), reads=[("lg", s)], writes=[("m8", s)])
                P.add("dve", lambda e, s=s: e.tensor_scalar(nm[s][:], m8[s][:, 0:1], -1.0, None, ALU.mult),
                      reads=[("m8", s)], writes=[("nm", s)])
                P.add("act", lambda e, s=s: e.activation(ex[s][:], lg[s][:], AF.Exp, bias=nm[s][:, 0:1], scale=1.0),
                      reads=[("lg", s), ("nm", s)], writes=[("ex", s)])
                P.add("dve", lambda e, s=s: e.tensor_scalar(mk[s][:], lg[s][:], m8[s][:, 3:4], None, ALU.is_ge),
                      reads=[("lg", s), ("m8", s)], writes=[("mk", s)])
                P.add("dve", lambda e, s=s: e.tensor_tensor(ex[s][:], ex[s][:], mk[s][:], ALU.mult),
                      reads=[("ex", s), ("mk", s)], writes=[("ex", s)])
                P.add("dve", lambda e, s=s: e.reduce_sum(sm[s][:], ex[s][:], AX.X), reads=[("ex", s)], writes=[("sm", s)])
                P.add("dve", lambda e, s=s: e.reciprocal(sm[s][:], sm[s][:]), reads=[("sm", s)], writes=[("sm", s)])
                P.add("dve", lambda e, s=s, i=i: e.tensor_scalar(gates[:, i, :], ex[s][:], sm[s][:, 0:1], None, ALU.mult),
                      reads=[("ex", s), ("sm", s)], writes=[("gates", i)])
                if C.ne == NE:
                    P.add("pe", lambda e, s=s, i=i: e.transpose(pl[s][0:NE, 128:256], gates[:, i, :], ident[:]),
                          reads=[("gates", i), "ident"], writes=[("pl", s)])
                    P.add("act", lambda e, s=s: e.activation(gT[s][:], pl[s][0:NE, 128:256], AF.Copy), reads=[("pl", s)], writes=[("gT", s)])
                    for hh in range(2):
                        b_ = s
                        P.add("pe", lambda e, s=s, b_=b_, hh=hh: e.matmul(pbias[b_][:], gT[s][:], bdall[:, hh * 512:(hh + 1) * 512], start=True, stop=True),
                              reads=[("gT", s), "bdall"], writes=[("pbias", b_)])
                        P.add("dve", lambda e, b_=b_, r=r, hh=hh: e.tensor_tensor(btmp[b_][:], pbias[b_][:], g2p[:, r, hh * 512:(hh + 1) * 512], ALU.mult),
                              reads=[("pbias", b_), "g2p"], writes=[("btmp", b_)])
                        P.add("pool", lambda e, b_=b_, i=i, hh=hh: e.tensor_tensor(XS[:, i, hh * 512:(hh + 1) * 512], XS[:, i, hh * 512:(hh + 1) * 512],
                                                                                    btmp[b_][:], ALU.add),
                              reads=[("btmp", b_), ("XS", i)], writes=[("XS", i)])
            P.interleave([(lambda i=i, tix=tix: pbody(i, tix)) for i, tix in enumerate(tiles)], C.ilv)
        P.barrier()
        import os
        if "nopre" in os.environ.get("SK", ""):
            return
        with ExitStack() as st3:
            NG = 4
            wgu = [C.sb(st3, "wgu", [128, 2, 8, 128], BF16) for _ in range(NG)]
            wdn = [C.sb(st3, "wdn", [128, 8, 1024], BF16) for _ in range(2)]
            ones1 = C.sb(st3, "ones1", [1, 128], F32)
            bgu = C.sb(st3, "bgu", [128, NE, 16], F32)
            g2b = C.sb(st3, "g2b", [128, 2, 1024], F32)
            actT = C.sb(st3, "actT", [128, 8, NTOK], BF16)
            gc = [C.sb(st3, "gc", [128, 512], F32) for _ in range(2)]
            sgm = [C.sb(st3, "sgm", [128, 512], F32) for _ in range(2)]
            u1 = [C.sb(st3, "u1", [128, 512], F32) for _ in range(2)]
            yt = [C.sb(st3, "yt", [128, 512], F32) for _ in range(2)]
            yt2 = [C.sb(st3, "yt2", [128, 512], F32) for _ in range(2)]
            pg = [C.ps(st3, "pg", [128, 512]) for _ in range(2)]
            pu = [C.ps(st3, "pu", [128, 512]) for _ in range(2)]
            py = [C.ps(st3, "py", [128, 512]) for _ in range(3)]
            for s_ in range(3):
                C.pskey(("pg", s_)); C.pskey(("pu", s_)); C.pskey(("py", s_))
            import os
            SK = os.environ.get("SK", "")
            if "ones1" not in SK:
                P.add("pool", lambda e: e.memset(ones1[:], 1.0), writes=["ones1"])
            if "bguld" not in SK:
                P.add("sp", lambda e: e.dma_start(out=bgu[:].rearrange("p e j -> p (e j)"), in_=io["b_guT"][layer]), writes=["bgu"], dma=True)
            import os
            SK = os.environ.get("SK", "")
            if "bguadd" not in SK:
                P.add("dve", lambda e: e.tensor_scalar(bgu[:, :, 8:16], bgu[:, :, 8:16], 1.0, None, ALU.add),
                      reads=["bgu"], writes=["bgu"])
            for r in range(2):
                if "g2b" in SK:
                    continue
                src = io["modrow"][layer, r:r + 1, 5 * 1024:6 * 1024].partition_broadcast(128)
                P.add("sp", lambda e, r=r, src=src: e.dma_start(out=g2b[:, r, :], in_=src), reads=["modrow"],
                      writes=["g2b"], dma=True)
            NS = 4
            wstg = [C.sb(st3, "wstg", [128, 2048], F32) for _ in range(NS)]
            nea = io["nea"]
            items = []
            for ex_ in range(C.ne):
                order = [("gu", 0), ("gu", 1), ("dn", 0), ("gu", 2), ("dn", 1), ("gu", 3), ("dn", 2), ("gu", 4), ("dn", 3),
                         ("gu", 5), ("gu", 6), ("gu", 7)]
                items += [(ex_, kind, q) for (kind, q) in order]
            pos = {it_: n_ for n_, it_ in enumerate(items)}
            state = {"staged": 0, "sti": 0, "ci": 0}
            slot_of = {}

            def stage_upto(n_):
                while state["staged"] <= min(n_, len(items) - 1):
                    ex2, kind, q = items[state["staged"]]
                    state["staged"] += 1
                    if C.nowdma and ex2 >= 2:
                        if kind == "gu":
                            slot_of[(ex2, q)] = state["ci"] % NG
                            state["ci"] += 1
                        continue
                    sg_ = state["sti"] % NS
                    state["sti"] += 1
                    if kind == "gu":
                        cs2 = state["ci"] % NG
                        state["ci"] += 1
                        slot_of[(ex2, q)] = cs2
                        src = io["w_guR"][(layer * nea + ex2) * 8 + q]
                        P.add("sp", lambda e, sg_=sg_, src=src: e.dma_start(out=wstg[sg_][:], in_=src), writes=[("wstg", sg_)], dma=True)
                        P.add("act", lambda e, sg_=sg_, cs2=cs2: e.activation(wgu[cs2][:].rearrange("p u k c -> p (u k c)"), wstg[sg_][:], AF.Copy),
                              reads=[("wstg", sg_)], writes=[("wgu", cs2)])
                    else:
                        ws2 = ex2 % 2
                        src = io["w_down"][layer * nea + ex2].rearrange("(j p) d -> p j d", p=128)[:, 2 * q:2 * q + 2, :]
                        P.add("sp", lambda e, sg_=sg_, src=src: e.dma_start(out=wstg[sg_][:].rearrange("p (j d) -> p j d", j=2), in_=src),
                              writes=[("wstg", sg_)], dma=True)
                        P.add("act", lambda e, sg_=sg_, ws2=ws2, q=q: e.activation(
                            wdn[ws2][:, 2 * q:2 * q + 2, :].rearrange("p j d -> p (j d)"), wstg[sg_][:], AF.Copy),
                            reads=[("wstg", sg_)], writes=[("wdn", ws2, q)])

            si = 0
            yi = 0
            pending_tail = []
            for ex_ in range(C.ne):
                ws = ex_ % 2
                for j in range(8):
                    stage_upto(pos[(ex_, "gu", j)] + 3)
                    cs_ = slot_of[(ex_, j)]
                    for (t0, nt) in groups:
                        w = nt * 128
                        c0 = t0 * 128
                        ss_ = si % 2
                        si += 1
                        for k in range(8):
                            P.add("pe", lambda e, cs_=cs_, k=k, ss_=ss_, c0=c0, w=w: e.matmul(
                                pg[ss_][:, 0:w], wgu[cs_][:, 0, k, :], fT[:, k, c0:c0 + w], start=(k == 0), stop=(k == 7)),
                                reads=[("wgu", cs_), "fT"], writes=[("pg", ss_)])
                        for k in range(8):
                            P.add("pe", lambda e, cs_=cs_, k=k, ss_=ss_, c0=c0, w=w: e.matmul(
                                pu[ss_][:, 0:w], wgu[cs_][:, 1, k, :], fT[:, k, c0:c0 + w], start=(k == 0), stop=(k == 7)),
                                reads=[("wgu", cs_), "fT"], writes=[("pu", ss_)])
                        P.add("dve", lambda e, ss_=ss_, w=w, ex_=ex_, j=j: e.tensor_scalar(
                            gc[ss_][:, 0:w], pg[ss_][:, 0:w], bgu[:, ex_, j:j + 1], 7.0, ALU.add, ALU.min),
                            reads=[("pg", ss_), "bgu"], writes=[("gc", ss_)])
                        P.add("act", lambda e, ss_=ss_, w=w: e.activation(sgm[ss_][:, 0:w], gc[ss_][:, 0:w], AF.Sigmoid, scale=1.702),
                              reads=[("gc", ss_)], writes=[("sgm", ss_)])
                        P.add("act", lambda e, ss_=ss_, w=w, ex_=ex_, j=j: e.activation(
                            u1[ss_][:, 0:w], pu[ss_][:, 0:w], AF.Identity, bias=bgu[:, ex_, 8 + j:9 + j], scale=1.0),
                            reads=[("pu", ss_), "bgu"], writes=[("u1", ss_)])
                        def tail(ss_=ss_, w=w, j=j, c0=c0):
                            P.add("pool", lambda e: e.tensor_tensor(sgm[ss_][:, 0:w], gc[ss_][:, 0:w], sgm[ss_][:, 0:w], ALU.mult),
                                  reads=[("gc", ss_), ("sgm", ss_)], writes=[("sgm", ss_)])
                            P.add("dve", lambda e: e.tensor_scalar(u1[ss_][:, 0:w], u1[ss_][:, 0:w], -6.0, 8.0, ALU.max, ALU.min),
                                  reads=[("u1", ss_)], writes=[("u1", ss_)])
                            P.add("dve", lambda e: e.tensor_tensor(actT[:, j, c0:c0 + w], sgm[ss_][:, 0:w], u1[ss_][:, 0:w], ALU.mult),
                                  reads=[("sgm", ss_), ("u1", ss_)], writes=[("actT", j)])
                        if pending_tail:
                            pending_tail.pop()()
                        pending_tail.append(tail)
                if pending_tail:
                    pending_tail.pop()()
                for i, tix in enumerate(tiles):
                    r = 1 if tix in ctx_tiles else 0
                    for hh in range(2):
                        ys = yi % 3
                        y2 = yi % 2
                        yi += 1
                        for j in range(8):
                            P.add("pe", lambda e, ys=ys, j=j, i=i, ws=ws, hh=hh: e.matmul(
                                py[ys][:], actT[:, j, i * 128:(i + 1) * 128], wdn[ws][:, j, hh * 512:(hh + 1) * 512],
                                start=(j == 0), stop=(j == 7)),
                                reads=[("actT", j), ("wdn", ws, j // 2)], writes=[("py", ys)])
                        P.add("act", lambda e, ys=ys, y2=y2, i=i, ex_=ex_: e.activation(
                            yt[y2][:], py[ys][:], AF.Copy, scale=gates[:, i, ex_:ex_ + 1]),
                            reads=[("py", ys), "gates"], writes=[("yt", y2)])
                        P.add("dve", lambda e, y2=y2, r=r, hh=hh: e.tensor_tensor(
                            yt2[y2][:], yt[y2][:], g2b[:, r, hh * 512:(hh + 1) * 512], ALU.mult),
                            reads=[("yt", y2), "g2b"], writes=[("yt2", y2)])
                        P.add("pool", lambda e, y2=y2, i=i, hh=hh: e.tensor_tensor(
                            XS[:, i, hh * 512:(hh + 1) * 512], XS[:, i, hh * 512:(hh + 1) * 512], yt2[y2][:], ALU.add),
                            reads=[("yt2", y2), ("XS", i, hh)], writes=[("XS", i, hh)])
            for i, tix in enumerate(tiles):
                if "wb" in SK:
                    continue
                if out_ap is not None:
                    if tix not in out_tiles:
                        continue
                    o = out_tiles[tix]
                    dst = out_ap[o * 128:(o + 1) * 128, :]
                    wkey = "OUT"
                else:
                    dst = XL[tix * 128:(tix + 1) * 128, :]
                    wkey = "XL"
                P.add("sp", lambda e, i=i, dst=dst: e.dma_start(out=dst, in_=XS[:, i, :]),
                      reads=[("XS", i, 0), ("XS", i, 1), ("XS", i)], writes=[wkey], dma=True)
    P.barrier()


def col_groups(n, w=512):
    out = []
    c = 0
    while c < n:
        out.append((c, min(w, n - c)))
        c += w
    return out


def mm_acc(C, out_ap, pskey, pairs, reads):
    n = len(pairs)
    for i, (l, r) in enumerate(pairs):
        C.P.add("pe", lambda e, l=l, r=r, i=i: e.matmul(out_ap, l, r, start=(i == 0), stop=(i == n - 1)),
                reads=reads, writes=[pskey], glue=(i < n - 1))


def evac(C, eng, out_ap, in_ap, reads, writes):
    if eng == "act":
        C.P.add("act", lambda e: e.activation(out_ap, in_ap, AF.Copy), reads=reads, writes=writes)
    else:
        C.P.add(eng, lambda e: e.tensor_copy(out_ap, in_ap), reads=reads, writes=writes)


def rstd_from_ss(C, rs, ss, n, key_ss, key_rs):
    C.P.add("act", lambda e: e.activation(rs, ss, AF.Sqrt, bias=EPS, scale=1.0 / n), reads=[key_ss], writes=[key_rs])
    C.P.add("dve", lambda e: e.reciprocal(rs, rs), reads=[key_rs], writes=[key_rs])


def phase_norm_all(C, io, st, layer, src, ntiles, ctx_tiles, hT, copy_to_xl):
    P = C.P
    ident = C.sb(st, "ident", [128, 128], F32)
    P.add("sp", lambda e: e.dma_start(out=ident[:], in_=io["ident"]), writes=["ident"], dma=True)
    with ExitStack() as st2:
        NB = NormBufs(C, st2, C.name("nb"))
        sc1, k1 = load_modcols(C, st2, io, layer, 1)
        sh1, k2 = load_modcols(C, st2, io, layer, 0)
        nm = C.sb(st2, "nm", [128, 8], F32)
        gm = C.sb(st2, "gm", [128, 2, 8], F32)
        P.add("sp", lambda e: e.dma_start(out=nm[:], in_=io["normT"][:, (2 * layer) * 8:(2 * layer + 1) * 8]),
              writes=["nm"], dma=True)
        for r in range(2):
            P.add("dve", lambda e, r=r: e.scalar_tensor_tensor(gm[:, r, :], sc1[:, r, :], 1.0, nm[:], ALU.add, ALU.mult),
                  reads=[k1, "nm"], writes=["gm1"])
        xt = [C.sb(st2, "xt", [128, 1024], F32) for _ in range(2)]

        def nbody(t):
            s = t % 2
            r = 1 if t in ctx_tiles else 0
            P.add("sp", lambda e, s=s, t=t: e.dma_start(out=xt[s][:], in_=src[t * 128:(t + 1) * 128, :]),
                  reads=["SRC"], writes=[("xt", s)], dma=True)
            if copy_to_xl and t < NT_OWN:
                P.add("sp", lambda e, s=s, t=t: e.dma_start(out=io["XL"][t * 128:(t + 1) * 128, :], in_=xt[s][:]),
                      reads=[("xt", s)], writes=["XL"], dma=True)
            norm_tile(C, NB, ident, xt[s][:], ("xt", s), gm[:, r, :], sh1[:, r, :], ["gm1", k2],
                      lambda k, t=t: hT[:, k, t * 128:(t + 1) * 128], "hT", s=s)
        P.interleave([(lambda t=t: nbody(t)) for t in range(ntiles)], C.ilv)
    P.barrier()
    return ident


def phase_inproj0(C, io):
    P = C.P
    with ExitStack() as st:
        hT = C.sb(st, "hT", [128, 8, NALL], BF16)
        phase_norm_all(C, io, st, 0, io["xall"], NT_ALL, {0, 1}, hT, True)
        win = C.sb(st, "win", [128, 8, 1440], BF16)
        wsrc = io["w_in"].rearrange("(k p) f -> p k f", p=128)
        for k in range(8):
            P.add("pool", lambda e, k=k: e.dma_start(out=win[:, k, :], in_=wsrc[:, k, :]), writes=["win"], dma=True)
        stg = [C.sb(st, "stg", [128, 512], F32) for _ in range(3)]
        pp = [C.ps(st, "pp", [128, 512]) for _ in range(3)]
        for s_ in range(3):
            C.pskey(("pp", s_))
        it = 0
        for (f0, ncols, dst) in ((0, NALL, io["xaT"]), (512, NOWN, io["gaT"])):
            for kc in range(4):
                for (c0, w) in col_groups(ncols):
                    s = it % 3
                    it += 1
                    mm_acc(C, pp[s][:, 0:w], ("pp", s),
                           [(win[:, k, f0 + kc * 128:f0 + (kc + 1) * 128], hT[:, k, c0:c0 + w]) for k in range(8)],
                           ["win", "hT"])
                    evac(C, "act" if it % 2 else "dve", stg[s][:, 0:w], pp[s][:, 0:w], [("pp", s)], [("stg", s)])
                    P.add("sp", lambda e, s=s, w=w, c0=c0, kc=kc, dst=dst: e.dma_start(
                        out=dst[kc * 128:(kc + 1) * 128, c0:c0 + w], in_=stg[s][:, 0:w]),
                        reads=[("stg", s)], writes=["fm_out"], dma=True)
        for t in range(NT_ALL):
            s = it % 3
            it += 1
            mm_acc(C, pp[s][:, 0:416], ("pp", s),
                   [(hT[:, k, t * 128:(t + 1) * 128], win[:, k, 1024:1440]) for k in range(8)], ["win", "hT"])
            evac(C, "act" if it % 2 else "dve", stg[s][:, 0:416], pp[s][:, 0:416], [("pp", s)], [("stg", s)])
            P.add("sp", lambda e, s=s, t=t: e.dma_start(out=io["qkr"][t * 128:(t + 1) * 128, :], in_=stg[s][:, 0:416]),
                  reads=[("stg", s)], writes=["qkr"], dma=True)
    P.barrier()


def phase_lru(C, io, ext=None):
    P = C.P
    SEG = ((0, NCTX), (NCTX, NALL))
    npb = 2 if ext is None else 1
    with (ExitStack() if ext is None else ext["cm"]) as st:
        if ext is not None:
            st = ext["st"]
        lv = C.sb(st, "lv", [128, 2, 4, 8], F32)
        cst = C.sb(st, "cst", [128, 2, 4, 2], F32)
        wbd = C.sb(st, "wbd", [128, 16, 128], F32)
        P.add("sp", lambda e: e.dma_start(out=lv[:].rearrange("p a b c -> p (a b c)"), in_=io["lruvec"]), writes=["lv"], dma=True)
        P.add("sp", lambda e: e.dma_start(out=wbd[:], in_=io["wbd"].rearrange("n c d -> c n d")), writes=["wbd"], dma=True)
        P.add("act", lambda e: e.activation(cst[:, :, :, 0], lv[:, :, :, 7], AF.Exp, scale=-1.0), reads=["lv"], writes=["cst"])
        P.add("act", lambda e: e.activation(cst[:, :, :, 0], cst[:, :, :, 0], AF.Ln, bias=1.0), reads=["cst"], writes=["cst"])
        P.add("dve", lambda e: e.tensor_scalar(cst[:, :, :, 1], cst[:, :, :, 0], -16.0, None, ALU.mult), reads=["cst"], writes=["cst2"])
        P.add("dve", lambda e: e.tensor_scalar(cst[:, :, :, 0], cst[:, :, :, 0], -8.0, None, ALU.mult), reads=["cst", "cst2"], writes=["cst"])
        xa = C.sb(st, "xa", [128, NALL], F32)
        xc = C.sb(st, "xc", [128, NALL], F32)
        ra = C.sb(st, "ra", [128, NALL], F32)
        gb = C.sb(st, "gb", [128, NALL], F32)
        e2 = C.sb(st, "e2", [128, NALL], F32)
        hh_ = [C.sb(st, "hf", [128, NALL], F32), C.sb(st, "hb", [128, NALL], F32)]
        ga = C.sb(st, "ga", [128, NOWN], F32)
        ya = C.sb(st, "ya", [128, NOWN], BF16)
        pr = [C.ps(st, "pr", [128, 512]) for _ in range(npb)]
        pi = [C.ps(st, "pi", [128, 512]) for _ in range(npb)]
        for s_ in range(2):
            C.pskey(("pr", s_))
            C.pskey(("pi", s_))
        it = 0
        for kc in range(4):
            P.add("sp", lambda e, kc=kc: e.dma_start(out=xa[:], in_=io["xaT"][kc * 128:(kc + 1) * 128, :]), writes=["xa"], dma=True)
            P.add("sp", lambda e, kc=kc: e.dma_start(out=ga[:], in_=io["gaT"][kc * 128:(kc + 1) * 128, :]), writes=["ga"], dma=True)
            for dl in range(2):
                V = lambda f, dl=dl, kc=kc: lv[:, dl, kc, f:f + 1]
                P.add("dve", lambda e, V=V: e.tensor_scalar(xc[:], xa[:], V(3), V(4), ALU.mult, ALU.add),
                      reads=["xa", "lv"], writes=["xc"])
                for j in range(3):
                    sft = 3 - j
                    for (a, b) in SEG:
                        if dl == 0:
                            o_sl, i_sl = (a + sft, b), (a, b - sft)
                        else:
                            o_sl, i_sl = (a, b - sft), (a + sft, b)
                        P.add("dve", lambda e, V=V, j=j, o_sl=o_sl, i_sl=i_sl: e.scalar_tensor_tensor(
                            xc[:, o_sl[0]:o_sl[1]], xa[:, i_sl[0]:i_sl[1]], V(j), xc[:, o_sl[0]:o_sl[1]], ALU.mult, ALU.add),
                            reads=["xa", "lv", "xc"], writes=["xc"])
                for (c0, w) in col_groups(NALL):
                    s = it % npb
                    it += 1
                    P.add("pe", lambda e, s=s, c0=c0, w=w, dl=dl, kc=kc: e.matmul(pr[s][:, 0:w], wbd[:, (0 * 2 + dl) * 4 + kc, :], xc[:, c0:c0 + w],
                                                                                start=True, stop=True),
                          reads=["wbd", "xc"], writes=[("pr", s)])
                    P.add("pe", lambda e, s=s, c0=c0, w=w, dl=dl, kc=kc: e.matmul(pi[s][:, 0:w], wbd[:, (1 * 2 + dl) * 4 + kc, :], xc[:, c0:c0 + w],
                                                                                start=True, stop=True),
                          reads=["wbd", "xc"], writes=[("pi", s)])
                    P.add("act", lambda e, s=s, c0=c0, w=w, V=V: e.activation(ra[:, c0:c0 + w], pr[s][:, 0:w], AF.Sigmoid, bias=V(5)),
                          reads=[("pr", s), "lv"], writes=["ra"])
                    P.add("act", lambda e, s=s, c0=c0, w=w, V=V: e.activation(gb[:, c0:c0 + w], pi[s][:, 0:w], AF.Sigmoid, bias=V(6)),
                          reads=[("pi", s), "lv"], writes=["gb"])
                cc = lambda f, dl=dl, kc=kc: cst[:, dl, kc, f:f + 1]
                P.add("act", lambda e, cc=cc: e.activation(e2[:], ra[:], AF.Exp, scale=cc(1)), reads=["ra", "cst", "cst2"], writes=["e2"])
                P.add("act", lambda e, cc=cc: e.activation(ra[:], ra[:], AF.Exp, scale=cc(0)), reads=["ra", "cst", "cst2", "e2"], writes=["ra"])
                P.add("act", lambda e: e.activation(e2[:], e2[:], AF.Sqrt, bias=1.0, scale=-1.0), reads=["e2"], writes=["e2"])
                P.add("dve", lambda e: e.tensor_tensor(gb[:], gb[:], xc[:], ALU.mult), reads=["gb", "xc"], writes=["gb"])
                P.add("pool", lambda e: e.tensor_tensor(gb[:], gb[:], e2[:], ALU.mult), reads=["gb", "e2"], writes=["gb"])
                h = hh_[dl]
                hk = ("h", dl)
                if dl == 0:
                    P.add("dve", lambda e, h=h: e.tensor_tensor_scan(h[:], ra[:], gb[:], 0.0, ALU.mult, ALU.add),
                          reads=["ra", "gb"], writes=[hk])
                else:
                    P.add("dve", lambda e, h=h: e.tensor_tensor_scan(h[:, NCTX - 1::-1], ra[:, NCTX - 1::-1], gb[:, NCTX - 1::-1], 0.0,
                                                                     ALU.mult, ALU.add),
                          reads=["ra", "gb"], writes=[hk])
                    P.add("dve", lambda e, h=h: e.tensor_tensor_scan(h[:, NALL - 1:NCTX - 1:-1], ra[:, NALL - 1:NCTX - 1:-1],
                                                                     gb[:, NALL - 1:NCTX - 1:-1], h[:, 0:1], ALU.mult, ALU.add),
                          reads=["ra", "gb", hk], writes=[hk])
            hf, hb = hh_
            P.add("pool", lambda e: e.tensor_tensor(hf[:, 0:NOWN], hf[:, 0:NOWN], hb[:, 0:NOWN], ALU.add),
                  reads=[("h", 0), ("h", 1)], writes=[("h", 0)])
            t1 = xc[:, 0:NOWN]
            P.add("dve", lambda e: e.tensor_tensor(t1, ga[:], ga[:], ALU.mult), reads=["ga", "xc"], writes=["xc"])
            P.add("dve", lambda e: e.tensor_scalar(t1, t1, 0.044715, 1.0, ALU.mult, ALU.add), reads=["xc"], writes=["xc"])
            P.add("dve", lambda e: e.tensor_tensor(t1, t1, ga[:], ALU.mult), reads=["xc", "ga"], writes=["xc"])
            P.add("act", lambda e: e.activation(t1, t1, AF.Sigmoid, scale=1.5957691216057308), reads=["xc"], writes=["xc"])
            P.add("dve", lambda e: e.tensor_tensor(t1, t1, ga[:], ALU.mult), reads=["xc", "ga"], writes=["xc"])
            P.add("dve", lambda e: e.tensor_tensor(ya[:], t1, hf[:, 0:NOWN], ALU.mult), reads=["xc", ("h", 0)], writes=["ya"])
            P.add("sp", lambda e, kc=kc: e.dma_start(out=io["mixT"][kc * 128:(kc + 1) * 128, :], in_=ya[:]),
                  reads=["ya"], writes=[("mixT", "lru")], dma=True)
    if ext is None:
        P.barrier()


def rope_apply(C, out, x, cs, tmp, G, h, reads, wkey, tkey):
    P = C.P
    cosb = cs[:, 0:h].unsqueeze(1).broadcast_to([128, G, h])
    sinb = cs[:, h:2 * h].unsqueeze(1).broadcast_to([128, G, h])
    x1, x2 = x[:, :, 0:h], x[:, :, h:2 * h]
    o1, o2 = out[:, :, 0:h], out[:, :, h:2 * h]
    t1, t2 = tmp[:, :, 0:h], tmp[:, :, h:2 * h]
    P.add("dve", lambda e: e.tensor_tensor(t1, x2, sinb, ALU.mult), reads=reads, writes=[tkey])
    P.add("dve", lambda e: e.tensor_tensor(t2, x1, sinb, ALU.mult), reads=reads, writes=[tkey])
    P.add("dve", lambda e: e.tensor_tensor(o1, x1, cosb, ALU.mult), reads=reads, writes=[wkey])
    P.add("dve", lambda e: e.tensor_tensor(o2, x2, cosb, ALU.mult), reads=reads, writes=[wkey])
    P.add("dve", lambda e: e.tensor_tensor(o1, o1, t1, ALU.subtract), reads=[wkey, tkey], writes=[wkey])
    P.add("dve", lambda e: e.tensor_tensor(o2, o2, t2, ALU.add), reads=[wkey, tkey], writes=[wkey])


def phase_mla_prep(C, io, ext=None):
    P = C.P
    with (ExitStack() if ext is None else ext["cm"]) as st:
        if ext is not None:
            st = ext["st"]
        ident = C.sb(st, "ident", [128, 128], F32)
        P.add("sp", lambda e: e.dma_start(out=ident[:], in_=io["ident"]), writes=["ident"], dma=True)
        wkvb = C.sb(st, "wkvb", [128, 1024], BF16)
        wqb = C.sb(st, "wqb", [128, 2, 768], BF16)
        P.add("pool", lambda e: e.dma_start(out=wkvb[:], in_=io["w_kv_b"]), writes=["wkvb"], dma=True)
        P.add("pool", lambda e: e.dma_start(out=wqb[:], in_=io["w_q_b"].rearrange("(c p) f -> p c f", p=128)), writes=["wqb"], dma=True)
        qan = C.sb(st, "qan", [128, 2], F32)
        kvan = C.sb(st, "kvan", [128, 1], F32)
        P.add("sp", lambda e: e.dma_start(out=qan[:], in_=io["q_a_normT"]), writes=["nrm"], dma=True)
        P.add("sp", lambda e: e.dma_start(out=kvan[:], in_=io["kv_a_normT"]), writes=["nrm"], dma=True)
        nn = C.sb(st, "nn", [128, 2, 64], F32)
        rn = C.sb(st, "rn", [128, 2, 32], F32)
        for i in range(2):
            P.add("sp", lambda e, i=i: e.dma_start(out=nn[:, i, :], in_=io["nope_norm"][i:i + 1, :].partition_broadcast(128)), writes=["nrm"], dma=True)
            P.add("sp", lambda e, i=i: e.dma_start(out=rn[:, i, :], in_=io["rope_norm"][i:i + 1, :].partition_broadcast(128)), writes=["nrm"], dma=True)
        gqk = C.sb(st, "gqk", [128, 64], F32)
        P.add("dve", lambda e: e.tensor_tensor(gqk[:], nn[:, 0, :], nn[:, 1, :], ALU.mult), reads=["nrm"], writes=["gqk"])
        junk = C.sb(st, "junk", [128, 256], F32)
        B2 = lambda nm_, shp, dt: [C.sb(st, nm_, shp, dt) for _ in range(2)]
        qk = B2("qk", [128, 416], F32)
        cs = B2("cs", [128, 32], F32)
        ssv = B2("ssv", [128, 4], F32)
        rsv = B2("rsv", [128, 4], F32)
        kvn = B2("kvn", [128, 128], F32)
        kvnT = B2("kvnT", [128, 128], BF16)
        sq = B2("sq", [128, 4, 96], F32)
        ssn = B2("ssn", [128, 24], F32)
        rsn = B2("rsn", [128, 24], F32)
        kcat = B2("kcat", [128, 8, 96], F32)
        qcat = B2("qcat", [128, 8, 96], F32)
        vaug = B2("vaug", [128, 8, 65], BF16)
        kr = B2("kr", [128, 1, 32], F32)
        kr2 = B2("kr2", [128, 1, 32], F32)
        rtmp = B2("rtmp", [128, 8, 32], F32)
        qr = B2("qr", [128, 8, 32], F32)
        qn = B2("qn", [128, 256], F32)
        qnT = B2("qnT", [128, 2, 128], BF16)
        ktile = B2("ktile", [96, 8, 128], BF16)
        qtile = B2("qtile", [96, 8, 128], BF16)
        pT = [C.ps(st, "pT", [128, 512]) for _ in range(1)]
        pkv = [C.ps(st, "pkv", [128, 512]) for _ in range(2)]
        pq = [C.ps(st, "pq", [128, 512]) for _ in range(2)]
        pT2 = [C.ps(st, "pT2", [128, 512]) for _ in range(2 if ext is None else 1)]
        if ext is not None:
            pT2 = [pT2[0], pT2[0]]
        T2K = (lambda b: ("pT2", b)) if ext is None else (lambda b: ("pT2", 0))
        for s_ in range(2):
            C.pskey(("pT", s_)); C.pskey(("pkv", s_)); C.pskey(("pq", s_)); C.pskey(("pT2", s_))
        for s_ in range(2):
            P.add("pool", lambda e, s_=s_: e.memset(vaug[s_][:], 1.0), writes=[("vaug", s_)])
        KTd = io["KT"].rearrange("h f t -> f h t")
        QTd = io["QT"].rearrange("h f t -> f h t")
        def bodyK(t):
            s = t % 2
            K = lambda n_, s=s: (n_, s)
            lat = t >= 2
            own = t < NT_OWN
            P.add("sp", lambda e, s=s, t=t: e.dma_start(out=qk[s][:], in_=io["qkr"][t * 128:(t + 1) * 128, :]), writes=[K("qk")], dma=True)
            if lat:
                P.add("sp", lambda e, s=s, t=t: e.dma_start(out=cs[s][:], in_=io["rope_mla"][(t - 2) * 128:(t - 1) * 128, :]),
                      writes=[K("cs")], dma=True)
            P.add("act", lambda e, s=s: e.activation(junk[:, 0:128], qk[s][:, 256:384], AF.Square, accum_out=ssv[s][:, 0:1]),
                  reads=[K("qk")], writes=["junk", K("ssv")])
            P.add("act", lambda e, s=s: e.activation(junk[:, 0:32], qk[s][:, 384:416], AF.Square, accum_out=ssv[s][:, 1:2]),
                  reads=[K("qk")], writes=["junk", K("ssv")])
            if own:
                P.add("act", lambda e, s=s: e.activation(junk[:, 0:256], qk[s][:, 0:256], AF.Square, accum_out=ssv[s][:, 2:3]),
                      reads=[K("qk")], writes=["junk", K("ssv")])
            for (c, n_) in ((0, 128), (1, 32)) + (((2, 256),) if own else ()):
                rstd_from_ss(C, rsv[s][:, c:c + 1], ssv[s][:, c:c + 1], n_, K("ssv"), K("rsv"))
            P.add("dve", lambda e, s=s: e.tensor_scalar(kvn[s][:], qk[s][:, 256:384], rsv[s][:, 0:1], None, ALU.mult),
                  reads=[K("qk"), K("rsv")], writes=[K("kvn")])
            P.add("pe", lambda e, s=s: e.transpose(pT[0][:, 0:128], kvn[s][:], ident[:]), reads=[K("kvn"), "ident"], writes=[("pT", 0)], glue=True)
            P.add("act", lambda e, s=s: e.activation(kvnT[s][:], pT[0][:, 0:128], AF.Copy, scale=kvan[:, 0:1]),
                  reads=[("pT", 0), "nrm"], writes=[K("kvnT")])
            for b in range(2):
                P.add("pe", lambda e, s=s, b=b: e.matmul(pkv[b][:], kvnT[s][:], wkvb[:, b * 512:(b + 1) * 512], start=True, stop=True),
                      reads=[K("kvnT"), "wkvb"], writes=[("pkv", b)])
                pv = pkv[b][:].rearrange("p (h c) -> p h c", h=4)
                P.add("act", lambda e, s=s, pv=pv: e.activation(sq[s][:, :, 0:64], pv[:, :, 0:64], AF.Square),
                      reads=[("pkv", b)], writes=[K("sq")])
                P.add("dve", lambda e, s=s, b=b: e.reduce_sum(ssn[s][:, 4 * b:4 * b + 4], sq[s][:, :, 0:64], AX.X),
                      reads=[K("sq")], writes=[K("ssn")])
                P.add("act", lambda e, s=s, b=b, pv=pv: e.activation(vaug[s][:, 4 * b:4 * b + 4, 0:64], pv[:, :, 64:128], AF.Copy),
                      reads=[("pkv", b)], writes=[K("vaug")])
            rstd_from_ss(C, rsn[s][:, 0:8], ssn[s][:, 0:8], 64, K("ssn"), K("rsn"))
            for b in range(2):
                pv = pkv[b][:].rearrange("p (h c) -> p h c", h=4)
                P.add("dve", lambda e, s=s, b=b, pv=pv: e.tensor_tensor(
                    kcat[s][:, 4 * b:4 * b + 4, 0:64], pv[:, :, 0:64],
                    rsn[s][:, 4 * b:4 * b + 4].unsqueeze(2).broadcast_to([128, 4, 64]), ALU.mult),
                    reads=[("pkv", b), K("rsn")], writes=[K("kcat")])
            P.add("dve", lambda e, s=s: e.tensor_scalar(kr[s][:, 0, :], qk[s][:, 384:416], rsv[s][:, 1:2], None, ALU.mult),
                  reads=[K("qk"), K("rsv")], writes=[K("kr")])
            P.add("dve", lambda e, s=s: e.tensor_tensor(kr[s][:, 0, :], kr[s][:, 0, :], rn[:, 1, :], ALU.mult),
                  reads=[K("kr"), "nrm"], writes=[K("kr")])
            if lat:
                rope_apply(C, kr2[s][:], kr[s][:], cs[s][:], rtmp[s][:, 0:1, :], 1, 16, [K("kr"), K("cs")], K("kr2"), K("rtmp"))
                ksrc, kkey = kr2[s], K("kr2")
            else:
                ksrc, kkey = kr[s], K("kr")
            P.add("pool", lambda e, s=s, ksrc=ksrc: e.tensor_copy(kcat[s][:, :, 64:96], ksrc[:, 0:1, :].broadcast_to([128, 8, 32])),
                  reads=[kkey], writes=[K("kcat")])
            for b in range(2):
                for hh in range(4):
                    h = 4 * b + hh
                    P.add("pe", lambda e, s=s, b=b, hh=hh, h=h: e.transpose(pT2[b][0:96, hh * 128:(hh + 1) * 128], kcat[s][:, h, :], ident[:]),
                          reads=[K("kcat"), "ident"], writes=[T2K(b)], glue=True)
                evac(C, "act" if b else "dve", ktile[s][:, 4 * b:4 * b + 4, :], pT2[b][0:96, :].rearrange("p (h c) -> p h c", h=4),
                     [T2K(b)], [K("ktile")])
            P.add("sp", lambda e, s=s, t=t: e.dma_start(out=KTd[:, :, t * 128:(t + 1) * 128], in_=ktile[s][:]),
                  reads=[K("ktile")], writes=["KT"], dma=True)
            P.add("sp", lambda e, s=s, t=t: e.dma_start(out=io["Vd"][t * 128:(t + 1) * 128, :], in_=vaug[s][:].rearrange("p h c -> p (h c)")),
                  reads=[K("vaug")], writes=["Vd"], dma=True)
        def bodyQ(t):
            s = t % 2
            K = lambda n_, s=s: (n_, s)
            lat = t >= 2
            P.add("dve", lambda e, s=s: e.tensor_scalar(qn[s][:], qk[s][:, 0:256], rsv[s][:, 2:3], None, ALU.mult),
                  reads=[K("qk"), K("rsv")], writes=[K("qn")])
            for c in range(2):
                P.add("pe", lambda e, s=s, c=c: e.transpose(pT[0][:, (c + 1) * 128:(c + 2) * 128], qn[s][:, c * 128:(c + 1) * 128], ident[:]),
                      reads=[K("qn"), "ident"], writes=[("pT", 0)], glue=True)
            for c in range(2):
                P.add("act", lambda e, s=s, c=c: e.activation(qnT[s][:, c, :], pT[0][:, (c + 1) * 128:(c + 2) * 128], AF.Copy, scale=qan[:, c:c + 1]),
                      reads=[("pT", 0), "nrm"], writes=[K("qnT")], glue=(c == 0))
            for b in range(2):
                mm_acc(C, pq[b][:, 0:384], ("pq", b), [(qnT[s][:, c, :], wqb[:, c, b * 384:(b + 1) * 384]) for c in range(2)],
                       [K("qnT"), "wqb"])
                pv = pq[b][:, 0:384].rearrange("p (h c) -> p h c", h=4)
                P.add("act", lambda e, s=s, pv=pv: e.activation(sq[s][:], pv, AF.Square), reads=[("pq", b)], writes=[K("sq")])
                P.add("dve", lambda e, s=s, b=b: e.reduce_sum(ssn[s][:, 8 + 4 * b:12 + 4 * b], sq[s][:, :, 0:64], AX.X),
                      reads=[K("sq")], writes=[K("ssn")])
                P.add("dve", lambda e, s=s, b=b: e.reduce_sum(ssn[s][:, 16 + 4 * b:20 + 4 * b], sq[s][:, :, 64:96], AX.X),
                      reads=[K("sq")], writes=[K("ssn")])
            rstd_from_ss(C, rsn[s][:, 8:16], ssn[s][:, 8:16], 64, K("ssn"), K("rsn"))
            rstd_from_ss(C, rsn[s][:, 16:24], ssn[s][:, 16:24], 32, K("ssn"), K("rsn"))
            for b in range(2):
                pv = pq[b][:, 0:384].rearrange("p (h c) -> p h c", h=4)
                P.add("dve", lambda e, s=s, b=b, pv=pv: e.tensor_tensor(
                    qcat[s][:, 4 * b:4 * b + 4, 0:64], pv[:, :, 0:64],
                    rsn[s][:, 8 + 4 * b:12 + 4 * b].unsqueeze(2).broadcast_to([128, 4, 64]), ALU.mult),
                    reads=[("pq", b), K("rsn")], writes=[K("qcat")])
                P.add("dve", lambda e, s=s, b=b, pv=pv: e.tensor_tensor(
                    qr[s][:, 4 * b:4 * b + 4, :], pv[:, :, 64:96],
                    rsn[s][:, 16 + 4 * b:20 + 4 * b].unsqueeze(2).broadcast_to([128, 4, 32]), ALU.mult),
                    reads=[("pq", b), K("rsn")], writes=[K("qr")])
            P.add("pool", lambda e, s=s: e.tensor_tensor(qcat[s][:, :, 0:64], qcat[s][:, :, 0:64],
                                                         gqk[:].unsqueeze(1).broadcast_to([128, 8, 64]), ALU.mult),
                  reads=[K("qcat"), "gqk"], writes=[K("qcat")])
            P.add("pool", lambda e, s=s: e.tensor_tensor(qr[s][:], qr[s][:], rn[:, 0, :].unsqueeze(1).broadcast_to([128, 8, 32]), ALU.mult),
                  reads=[K("qr"), "nrm"], writes=[K("qr")])
            if lat:
                rope_apply(C, qcat[s][:, :, 64:96], qr[s][:], cs[s][:], rtmp[s][:], 8, 16, [K("qr"), K("cs")], K("qcat"), K("rtmp"))
            else:
                P.add("pool", lambda e, s=s: e.tensor_copy(qcat[s][:, :, 64:96], qr[s][:]), reads=[K("qr")], writes=[K("qcat")])
            for b in range(2):
                for hh in range(4):
                    h = 4 * b + hh
                    P.add("pe", lambda e, s=s, b=b, hh=hh, h=h: e.transpose(pT2[b][0:96, hh * 128:(hh + 1) * 128], qcat[s][:, h, :], ident[:]),
                          reads=[K("qcat"), "ident"], writes=[T2K(b)], glue=True)
                evac(C, "act" if b else "dve", qtile[s][:, 4 * b:4 * b + 4, :], pT2[b][0:96, :].rearrange("p (h c) -> p h c", h=4),
                     [T2K(b)], [K("qtile")])
            P.add("sp", lambda e, s=s, t=t: e.dma_start(out=QTd[:, :, t * 128:(t + 1) * 128], in_=qtile[s][:]),
                  reads=[K("qtile")], writes=["QT"], dma=True)
        for t in range(NT_ALL + 1):
            bl = []
            if t < NT_ALL:
                bl.append(lambda t=t: bodyK(t))
            if 1 <= t <= NT_OWN:
                bl.append(lambda t=t: bodyQ(t - 1))
            P.interleave(bl, C.ilv)
    if ext is None:
        P.barrier()


def phase_mla_attn(C, io):
    P = C.P
    scale = 96.0 ** -0.5
    with ExitStack() as st:
        QTs = C.sb(st, "QTs", [96, 8, NOWN], BF16)
        P.add("sp", lambda e: e.dma_start(out=QTs[:], in_=io["QT"].rearrange("h f t -> f h t")), writes=["QTs"], dma=True)
        KTh = [C.sb(st, "KTh", [96, NALL], BF16) for _ in range(2)]
        Vall = C.sb(st, "Vall", [128, NT_ALL, 520], BF16)
        Vsrc = io["Vd"].rearrange("(t p) c -> p t c", p=128)
        for t0 in range(0, NT_ALL, 9):
            t1 = min(NT_ALL, t0 + 9)
            P.add("sp", lambda e, t0=t0, t1=t1: e.dma_start(out=Vall[:, t0:t1, :], in_=Vsrc[:, t0:t1, :]), writes=["Vall"], dma=True)
        ones65 = C.sb(st, "ones65", [65, 64], F32)
        P.add("pool", lambda e: e.memset(ones65[:], 1.0), writes=["ones65"])
        pt = [C.sb(st, "ptx", [128, 512], BF16) for _ in range(3)]
        rden = [C.sb(st, "rden", [65, 512], F32) for _ in range(2)]
        bcs = [C.sb(st, "bcs", [64, 512], F32) for _ in range(2)]
        ybt = [C.sb(st, "ybt", [64, 512], BF16) for _ in range(2)]
        ps_s = [C.ps(st, "ps_s", [128, 512]) for _ in range(3)]
        po = [C.ps(st, "po", [128, 512]) for _ in range(2)]
        pb = [C.ps(st, "pb", [128, 512]) for _ in range(2)]
        for s_ in range(3):
            C.pskey(("ps_s", s_)); C.pskey(("po", s_)); C.pskey(("pb", s_))
        qgroups = [(0, NCTX, [0, 1])] + [(NCTX + c0, w, list(range(NT_ALL))) for (c0, w) in col_groups(NOWN - NCTX)]
        si = 0
        gi = 0
        for h in range(8):
            hs = h % 2
            P.add("sp", lambda e, h=h, hs=hs: e.dma_start(out=KTh[hs][:], in_=io["KT"][h]), writes=[("KTh", hs)], dma=True)
            for (c0, w, kts) in qgroups:
                g = gi % 2
                gi += 1
                n = len(kts)
                base = si
                si += n

                def S_mm(i, base=base, hs=hs, h=h, c0=c0, w=w, kts=kts):
                    s = (base + i) % 3
                    kt = kts[i]
                    P.add("pe", lambda e: e.matmul(ps_s[s][:, 0:w], KTh[hs][:, kt * 128:(kt + 1) * 128], QTs[:, h, c0:c0 + w], start=True, stop=True),
                          reads=[("KTh", hs), "QTs"], writes=[("ps_s", s)])
                for i in range(min(2, n)):
                    S_mm(i)
                for i, kt in enumerate(kts):
                    s = (base + i) % 3
                    P.add("act", lambda e, s=s, w=w: e.activation(pt[s][:, 0:w], ps_s[s][:, 0:w], AF.Exp, scale=scale),
                          reads=[("ps_s", s)], writes=[("ptx", s)])
                    if i + 2 < n:
                        S_mm(i + 2)
                    P.add("pe", lambda e, s=s, g=g, kt=kt, w=w, i=i, n=n, h=h: e.matmul(
                        po[g][0:65, 0:w], Vall[:, kt, h * 65:(h + 1) * 65], pt[s][:, 0:w], start=(i == 0), stop=(i == n - 1)),
                        reads=["Vall", ("ptx", s)], writes=[("po", g)])
                P.add("dve", lambda e, g=g, w=w: e.reciprocal(rden[g][64:65, 0:w], po[g][64:65, 0:w]), reads=[("po", g)], writes=[("rden", g)])
                P.add("pe", lambda e, g=g, w=w: e.matmul(pb[g][0:64, 0:w], ones65[64:65, :], rden[g][64:65, 0:w], start=True, stop=True),
                      reads=["ones65", ("rden", g)], writes=[("pb", g)])
                P.add("act", lambda e, g=g, w=w: e.activation(bcs[g][:, 0:w], pb[g][0:64, 0:w], AF.Copy), reads=[("pb", g)], writes=[("bcs", g)])
                P.add("dve", lambda e, g=g, w=w: e.tensor_tensor(ybt[g][:, 0:w], po[g][0:64, 0:w], bcs[g][:, 0:w], ALU.mult),
                      reads=[("po", g), ("bcs", g)], writes=[("ybt", g)])
                P.add("sp", lambda e, g=g, w=w, c0=c0, h=h: e.dma_start(out=io["mixT"][512 + h * 64:512 + (h + 1) * 64, c0:c0 + w], in_=ybt[g][:, 0:w]),
                      reads=[("ybt", g)], writes=["mixT"], dma=True)
    P.barrier()


def phase_outproj(C, io, layer, wname, mixT, ncols, xl_tiles, ctx_tiles):
    P = C.P
    with ExitStack() as st:
        mix = C.sb(st, "mix", [128, 8, ncols], BF16)
        P.add("sp", lambda e: e.dma_start(out=mix[:], in_=mixT.rearrange("(k p) t -> p k t", p=128)), writes=["mix"], dma=True)
        wo = C.sb(st, "wo", [128, 8, 1024], BF16)
        wsrc = io[wname].rearrange("(k p) d -> p k d", p=128)
        for k in range(0, 8, 2):
            P.add("pool", lambda e, k=k: e.dma_start(out=wo[:, k:k + 2, :], in_=wsrc[:, k:k + 2, :]), writes=["wo"], dma=True)
        g1b = C.sb(st, "g1b", [128, 2, 1024], F32)
        for r in range(2):
            src = io["modrow"][layer, r:r + 1, 2 * 1024:3 * 1024].partition_broadcast(128)
            P.add("sp", lambda e, r=r, src=src: e.dma_start(out=g1b[:, r, :], in_=src), writes=["g1b"], dma=True)
        xt = [C.sb(st, "xt", [128, 1024], F32) for _ in range(2)]
        tmp = [C.sb(st, "tmp", [128, 512], F32) for _ in range(2)]
        po = [C.ps(st, "po", [128, 512]) for _ in range(3)]
        for s_ in range(3):
            C.pskey(("po", s_))
        pi_ = 0
        for i, tix in enumerate(xl_tiles):
            s = i % 2
            r = 1 if tix in ctx_tiles else 0
            P.add("sp", lambda e, s=s, tix=tix: e.dma_start(out=xt[s][:], in_=io["XL"][tix * 128:(tix + 1) * 128, :]),
                  reads=[("XL", tix)], writes=[("xt", s, 0), ("xt", s, 1)], dma=True)
            for hh in range(2):
                p_ = pi_ % 3
                t_ = pi_ % 2
                pi_ += 1
                mm_acc(C, po[p_][:], ("po", p_), [(mix[:, k, i * 128:(i + 1) * 128], wo[:, k, hh * 512:(hh + 1) * 512]) for k in range(8)],
                       ["mix", "wo"])
                P.add("dve", lambda e, p_=p_, t_=t_, r=r, hh=hh: e.tensor_tensor(tmp[t_][:], po[p_][:], g1b[:, r, hh * 512:(hh + 1) * 512], ALU.mult),
                      reads=[("po", p_), "g1b"], writes=[("tmp", t_)])
                P.add("pool", lambda e, s=s, t_=t_, hh=hh: e.tensor_tensor(xt[s][:, hh * 512:(hh + 1) * 512], xt[s][:, hh * 512:(hh + 1) * 512],
                                                                            tmp[t_][:], ALU.add),
                      reads=[("tmp", t_), ("xt", s, hh)], writes=[("xt", s, hh)])
            P.add("sp", lambda e, s=s, tix=tix: e.dma_start(out=io["XL"][tix * 128:(tix + 1) * 128, :], in_=xt[s][:]),
                  reads=[("xt", s, 0), ("xt", s, 1)], writes=[("XL", tix)], dma=True)
    P.barrier()


def phase_qkv1(C, io):
    P = C.P
    with ExitStack() as st:
        hT = C.sb(st, "hT1", [128, 8, NOWN], BF16)
        ident = phase_norm_all(C, io, st, 1, io["XL"], NT_OWN, {0, 1}, hT, False)
        wq = C.sb(st, "wqkv", [128, 8, 1536], BF16)
        wsrc = io["w_qkv"].rearrange("(k p) f -> p k f", p=128)
        for k in range(8):
            P.add("pool", lambda e, k=k: e.dma_start(out=wq[:, k, :], in_=wsrc[:, k, :]), writes=["wqkv"], dma=True)
        qkn = C.sb(st, "qkn", [128, 2, 64], F32)
        for i in range(2):
            P.add("sp", lambda e, i=i: e.dma_start(out=qkn[:, i, :], in_=io["qk_norm"][i:i + 1, :].partition_broadcast(128)), writes=["qkn"], dma=True)
        B2 = lambda nm_, shp, dt: [C.sb(st, nm_, shp, dt) for _ in range(2)]
        cs = B2("cs1", [128, 64], F32)
        sq = B2("sq1", [128, 8, 64], F32)
        ss = B2("ss1", [128, 24], F32)
        rs = B2("rs1", [128, 24], F32)
        kn = B2("kn1", [128, 4, 64], F32)
        kcat = B2("kcat1", [128, 4, 64], F32)
        qn = B2("qn1", [128, 16, 64], F32)
        qcat = B2("qcat1", [128, 16, 64], F32)
        rtmp = B2("rtmp1", [128, 16, 64], F32)
        vaug = B2("vaug1", [128, 4, 65], BF16)
        ktile = B2("ktile1", [64, 4, 128], BF16)
        qtile = B2("qtile1", [64, 16, 128], BF16)
        pq = [C.ps(st, "pq1", [128, 512]) for _ in range(3)]
        pT2 = [C.ps(st, "pT21", [128, 512]) for _ in range(2)]
        for s_ in range(3):
            C.pskey(("pq1", s_)); C.pskey(("pT21", s_))
        for s_ in range(2):
            P.add("pool", lambda e, s_=s_: e.memset(vaug[s_][:], 1.0), writes=[("vaug1", s_)])
        KTd = io["KT1"].rearrange("g f t -> f g t")
        QTd = io["QT1"].rearrange("g f (l j c) -> f g l j c", l=16, j=4)
        def bodyK(t):
            s = t % 2
            K = lambda n_, s=s: (n_, s)
            lat = t >= 2
            if lat:
                P.add("sp", lambda e, s=s, t=t: e.dma_start(out=cs[s][:], in_=io["rope_gqa"][(t - 2) * 128:(t - 1) * 128, :]),
                      writes=[K("cs1")], dma=True)
            mm_acc(C, pq[2][:], ("pq1", 2), [(hT[:, k, t * 128:(t + 1) * 128], wq[:, k, 1024:1536]) for k in range(8)], ["hT", "wqkv"])
            kv = pq[2][:].rearrange("p (h c) -> p h c", h=8)
            P.add("act", lambda e, s=s, kv=kv: e.activation(sq[s][:, 0:4, :], kv[:, 0:4, :], AF.Square), reads=[("pq1", 2)], writes=[K("sq1")])
            P.add("dve", lambda e, s=s: e.reduce_sum(ss[s][:, 0:4], sq[s][:, 0:4, :], AX.X), reads=[K("sq1")], writes=[K("ss1")])
            P.add("act", lambda e, s=s, kv=kv: e.activation(vaug[s][:, :, 0:64], kv[:, 4:8, :], AF.Copy), reads=[("pq1", 2)], writes=[K("vaug1")])
            rstd_from_ss(C, rs[s][:, 0:4], ss[s][:, 0:4], 64, K("ss1"), K("rs1"))
            P.add("dve", lambda e, s=s, kv=kv: e.tensor_tensor(kn[s][:], kv[:, 0:4, :], rs[s][:, 0:4].unsqueeze(2).broadcast_to([128, 4, 64]), ALU.mult),
                  reads=[("pq1", 2), K("rs1")], writes=[K("kn1")])
            P.add("pool", lambda e, s=s: e.tensor_tensor(kn[s][:], kn[s][:], qkn[:, 1, :].unsqueeze(1).broadcast_to([128, 4, 64]), ALU.mult),
                  reads=[K("kn1"), "qkn"], writes=[K("kn1")])
            if lat:
                rope_apply(C, kcat[s][:], kn[s][:], cs[s][:], rtmp[s][:, 0:4, :], 4, 32, [K("kn1"), K("cs1")], K("kcat1"), K("rtmp1"))
                ksrc, kkey = kcat[s], K("kcat1")
            else:
                ksrc, kkey = kn[s], K("kn1")
            for g in range(4):
                P.add("pe", lambda e, ksrc=ksrc, g=g: e.transpose(pT2[0][0:64, g * 128:(g + 1) * 128], ksrc[:, g, :], ident[:]),
                      reads=[kkey, "ident"], writes=[("pT21", 0)], glue=True)
            evac(C, "dve", ktile[s][:], pT2[0][0:64, :].rearrange("p (h c) -> p h c", h=4), [("pT21", 0)], [K("ktile1")])
            P.add("sp", lambda e, s=s, t=t: e.dma_start(out=KTd[:, :, t * 128:(t + 1) * 128], in_=ktile[s][:]), reads=[K("ktile1")], writes=["KT1"], dma=True)
            P.add("sp", lambda e, s=s, t=t: e.dma_start(out=io["V1d"][t * 128:(t + 1) * 128, :], in_=vaug[s][:].rearrange("p h c -> p (h c)")),
                  reads=[K("vaug1")], writes=["V1d"], dma=True)
        def bodyQ(t):
            s = t % 2
            K = lambda n_, s=s: (n_, s)
            for b in range(2):
                mm_acc(C, pq[b][:], ("pq1", b), [(hT[:, k, t * 128:(t + 1) * 128], wq[:, k, b * 512:(b + 1) * 512]) for k in range(8)], ["hT", "wqkv"])
                qv = pq[b][:].rearrange("p (h c) -> p h c", h=8)
                P.add("act", lambda e, s=s, qv=qv: e.activation(sq[s][:], qv, AF.Square), reads=[("pq1", b)], writes=[K("sq1")])
                P.add("dve", lambda e, s=s, b=b: e.reduce_sum(ss[s][:, 8 + 8 * b:16 + 8 * b], sq[s][:], AX.X), reads=[K("sq1")], writes=[K("ss1")])
            rstd_from_ss(C, rs[s][:, 8:24], ss[s][:, 8:24], 64, K("ss1"), K("rs1"))
            for b in range(2):
                qv = pq[b][:].rearrange("p (h c) -> p h c", h=8)
                P.add("dve", lambda e, s=s, b=b, qv=qv: e.tensor_tensor(
                    qn[s][:, 8 * b:8 * b + 8, :], qv, rs[s][:, 8 + 8 * b:16 + 8 * b].unsqueeze(2).broadcast_to([128, 8, 64]), ALU.mult),
                    reads=[("pq1", b), K("rs1")], writes=[K("qn1")])
            P.add("pool", lambda e, s=s: e.tensor_tensor(qn[s][:], qn[s][:], qkn[:, 0, :].unsqueeze(1).broadcast_to([128, 16, 64]), ALU.mult),
                  reads=[K("qn1"), "qkn"], writes=[K("qn1")])
            rope_apply(C, qcat[s][:], qn[s][:], cs[s][:], rtmp[s][:], 16, 32, [K("qn1"), K("cs1")], K("qcat1"), K("rtmp1"))
            for g in range(4):
                b = g % 2
                for j in range(4):
                    P.add("pe", lambda e, s=s, b=b, g=g, j=j: e.transpose(pT2[b][0:64, j * 128:(j + 1) * 128], qcat[s][:, g * 4 + j, :], ident[:]),
                          reads=[K("qcat1"), "ident"], writes=[("pT21", b)], glue=True)
                evac(C, "act" if b else "dve", qtile[s][:, g * 4:(g + 1) * 4, :], pT2[b][0:64, :].rearrange("p (h c) -> p h c", h=4),
                     [("pT21", b)], [K("qtile1")])
            lt = t - 2
            for g in range(4):
                P.add("sp", lambda e, s=s, lt=lt, g=g: e.dma_start(out=QTd[:, g, lt, :, :], in_=qtile[s][:, g * 4:(g + 1) * 4, :]),
                      reads=[K("qtile1")], writes=["QT1"], dma=True)
        for t in range(NT_OWN + 1):
            bl = []
            if t < NT_OWN:
                bl.append(lambda t=t: bodyK(t))
            if 2 <= t - 1 < 18:
                bl.append(lambda t=t: bodyQ(t - 1))
            P.interleave(bl, C.ilv)
    P.barrier()


def phase_attn1(C, io):
    P = C.P
    scale = 64.0 ** -0.5
    with ExitStack() as st:
        KTs = C.sb(st, "KT1s", [64, 4, NOWN], BF16)
        P.add("sp", lambda e: e.dma_start(out=KTs[:], in_=io["KT1"].rearrange("g f t -> f g t")), writes=["KT1s"], dma=True)
        V1 = C.sb(st, "V1s", [128, NT_OWN, 260], BF16)
        P.add("sp", lambda e: e.dma_start(out=V1[:], in_=io["V1d"].rearrange("(t p) c -> p t c", p=128)), writes=["V1s"], dma=True)
        QTs = C.sb(st, "QT1s", [64, 4, 16 * 512], BF16)
        for g in range(4):
            P.add("sp", lambda e, g=g: e.dma_start(out=QTs[:, g, :], in_=io["QT1"][g]), writes=["QT1s"], dma=True)
        msk = C.sb(st, "msk", [128, 2, 128], F32)
        P.add("sp", lambda e: e.dma_start(out=msk[:], in_=io["masks"].rearrange("m k q -> k m q")), writes=["msk"], dma=True)
        snk = C.sb(st, "snk", [65, 4, 512], F32)
        P.add("sp", lambda e: e.dma_start(out=snk[64:65, :, :], in_=io["sink_rep"].rearrange("(o g) c -> o g c", o=1)), writes=["snk"], dma=True)
        P.add("act", lambda e: e.activation(snk[64:65, :, :], snk[64:65, :, :], AF.Exp), reads=["snk"], writes=["snk"])
        ones65 = C.sb(st, "ones65", [65, 64], F32)
        P.add("pool", lambda e: e.memset(ones65[:], 1.0), writes=["ones65"])
        pt = [C.sb(st, "ptx", [128, 512], BF16) for _ in range(3)]
        rden = [C.sb(st, "rden", [65, 512], F32) for _ in range(2)]
        bcs = [C.sb(st, "bcs", [64, 512], F32) for _ in range(2)]
        ybt = [C.sb(st, "ybt", [64, 512], BF16) for _ in range(2)]
        ps_s = [C.ps(st, "ps_s", [128, 512]) for _ in range(3)]
        po = [C.ps(st, "po", [128, 512]) for _ in range(2)]
        pb = [C.ps(st, "pb", [128, 512]) for _ in range(2)]
        for s_ in range(3):
            C.pskey(("ps_s", s_)); C.pskey(("po", s_)); C.pskey(("pb", s_))
        Md = io["mixT1"].rearrange("(h f) t -> f h t", f=64)
        si = 0
        gi = 0
        for lt in range(16):
            kts = ([(lt + 1, 0)] if lt >= 1 else []) + [(lt + 2, None), (lt + 3, 1), (0, None), (1, None)]
            for g in range(4):
                gg = gi % 2
                gi += 1
                n = len(kts)
                base = si
                si += n

                def S_mm(i, base=base, g=g, lt=lt, kts=kts):
                    s = (base + i) % 3
                    kt = kts[i][0]
                    P.add("pe", lambda e: e.matmul(ps_s[s][:], KTs[:, g, kt * 128:(kt + 1) * 128], QTs[:, g, lt * 512:(lt + 1) * 512], start=True, stop=True),
                          reads=["KT1s", "QT1s"], writes=[("ps_s", s)])
                for i in range(min(2, n)):
                    S_mm(i)
                for i, (kt, m) in enumerate(kts):
                    s = (base + i) % 3
                    P.add("act", lambda e, s=s: e.activation(pt[s][:], ps_s[s][:], AF.Exp, scale=scale), reads=[("ps_s", s)], writes=[("ptx", s)])
                    if m is not None:
                        P.add("dve", lambda e, s=s, m=m: e.tensor_tensor(
                            pt[s][:].rearrange("p (j c) -> p j c", j=4), pt[s][:].rearrange("p (j c) -> p j c", j=4),
                            msk[:, m, :].unsqueeze(1).broadcast_to([128, 4, 128]), ALU.mult),
                            reads=[("ptx", s), "msk"], writes=[("ptx", s)])
                    if i + 2 < n:
                        S_mm(i + 2)
                    P.add("pe", lambda e, s=s, gg=gg, kt=kt, g=g, i=i, n=n: e.matmul(
                        po[gg][0:65, :], V1[:, kt, g * 65:(g + 1) * 65], pt[s][:], start=(i == 0), stop=(i == n - 1)),
                        reads=["V1s", ("ptx", s)], writes=[("po", gg)])
                P.add("dve", lambda e, gg=gg, g=g: e.tensor_tensor(rden[gg][64:65, :], po[gg][64:65, :], snk[64:65, g, :], ALU.add),
                      reads=[("po", gg), "snk"], writes=[("rden", gg)])
                P.add("dve", lambda e, gg=gg: e.reciprocal(rden[gg][64:65, :], rden[gg][64:65, :]), reads=[("rden", gg)], writes=[("rden", gg)])
                P.add("pe", lambda e, gg=gg: e.matmul(pb[gg][0:64, :], ones65[64:65, :], rden[gg][64:65, :], start=True, stop=True),
                      reads=["ones65", ("rden", gg)], writes=[("pb", gg)])
                P.add("act", lambda e, gg=gg: e.activation(bcs[gg][:], pb[gg][0:64, :], AF.Copy), reads=[("pb", gg)], writes=[("bcs", gg)])
                P.add("dve", lambda e, gg=gg: e.tensor_tensor(ybt[gg][:], po[gg][0:64, :], bcs[gg][:], ALU.mult),
                      reads=[("po", gg), ("bcs", gg)], writes=[("ybt", gg)])
                P.add("sp", lambda e, gg=gg, g=g, lt=lt: e.dma_start(out=Md[:, g * 4:(g + 1) * 4, lt * 128:(lt + 1) * 128],
                                                                    in_=ybt[gg][:].rearrange("p (j c) -> p j c", j=4)),
                      reads=[("ybt", gg)], writes=["mixT1"], dma=True)
    P.barrier()


IN_SPECS = {
    "xall": ([NALL, D], F32),
    "cT": ([128, 16], F32),
    "w_mod": ([2, D, 6 * D], F32),
    "b_mod": ([2, 6 * D], F32),
    "normT": ([128, 32], F32),
    "ident": ([128, 128], F32),
    "w_in": ([D, 1440], F32),
    "lruvec": ([128, 64], F32),
    "wbd": ([16, 128, 128], F32),
    "q_a_normT": ([128, 2], F32),
    "kv_a_normT": ([128, 1], F32),
    "w_q_b": ([256, 768], F32),
    "w_kv_b": ([128, 1024], F32),
    "nope_norm": ([2, 64], F32),
    "rope_norm": ([2, 32], F32),
    "rope_mla": ([SEQ, 32], F32),
    "w_out_even": ([D, D], F32),
    "w_qkv": ([D, 1536], F32),
    "qk_norm": ([2, 64], F32),
    "sink_rep": ([4, 512], F32),
    "w_out_odd": ([D, D], F32),
    "rope_gqa": ([2176, 64], F32),
    "masks": ([2, 128, 128], F32),
    "w_router": ([2, D, NE], F32),
    "b_router": ([2, NE], F32),
    "w_guR": ([2 * NE * 8, 128, 2048], F32),
    "b_guT": ([2, 128, NE * 16], F32),
    "w_down": ([2 * NE, D, D], F32),
    "b_down": ([2 * NE, D], F32),
}


def build_program(cfg):
    nc = bass.Bass("TRN2", target_bir_lowering=False)
    io = {}
    used = cfg.get("inputs", list(IN_SPECS))
    nea = cfg.get("ne_alloc", NE)
    for n in used:
        shp, dt = IN_SPECS[n]
        shp = [2 * nea if (v == 2 * NE and n in ("w_down", "b_down")) else (2 * nea * 8 if (v == 2 * NE * 8 and n == "w_guR") else v) for v in shp]
        io[n] = nc.dram_tensor(n, shp, dt, kind="ExternalInput").ap()
    io["nea"] = nea
    io["modrow"] = nc.dram_tensor("modrow", [2, 2, 6 * D], F32, kind="Internal").ap()
    io["XL"] = nc.dram_tensor("XL", [NOWN, D], F32, kind="Internal").ap()
    io["xaT"] = nc.dram_tensor("xaT", [512, NALL], F32, kind="Internal").ap()
    io["gaT"] = nc.dram_tensor("gaT", [512, NOWN], F32, kind="Internal").ap()
    io["qkr"] = nc.dram_tensor("qkr", [NALL, 416], F32, kind="Internal").ap()
    io["mixT"] = nc.dram_tensor("mixT", [D, NOWN], BF16, kind="Internal").ap()
    io["QT"] = nc.dram_tensor("QT", [8, 96, NOWN], BF16, kind="Internal").ap()
    io["KT"] = nc.dram_tensor("KT", [8, 96, NALL], BF16, kind="Internal").ap()
    io["Vd"] = nc.dram_tensor("Vd", [NALL, 520], BF16, kind="Internal").ap()
    io["mixT1"] = nc.dram_tensor("mixT1", [D, 2048], BF16, kind="Internal").ap()
    io["QT1"] = nc.dram_tensor("QT1", [4, 64, 16 * 512], BF16, kind="Internal").ap()
    io["KT1"] = nc.dram_tensor("KT1", [4, 64, NOWN], BF16, kind="Internal").ap()
    io["V1d"] = nc.dram_tensor("V1d", [NOWN, 260], BF16, kind="Internal").ap()
    io["out"] = nc.dram_tensor("out", [2048, D], F32, kind="ExternalOutput").ap()
    if cfg.get("dbg_xlin"):
        io["XLin"] = nc.dram_tensor("XLin", [NOWN, D], F32, kind="ExternalInput").ap()
    if cfg.get("dbg_xlout"):
        io["XLout"] = nc.dram_tensor("XLout", [NOWN, D], F32, kind="ExternalOutput").ap()
    if cfg.get("dbg_mod"):
        io["modout"] = nc.dram_tensor("modout", [2, 2, 6 * D], F32, kind="ExternalOutput").ap()
    with ExitStack() as stack:
        P = Prog(nc, stack)
        C = Ctx(nc, P)
        C.ne = cfg.get("ne", NE)
        C.lvl = cfg.get("lvl", 9)
        C.ilv = cfg.get("ilv", 2)
        C.nowdma = cfg.get("nowdma", 0)
        phases = cfg["phases"]
        if cfg.get("dbg_xlin"):
            for t in range(NT_OWN):
                P.add("sp", lambda e, t=t: e.dma_start(out=io["XL"][t * 128:(t + 1) * 128, :], in_=io["XLin"][t * 128:(t + 1) * 128, :]),
                      writes=["XL"], dma=True)
            P.barrier()
        if "mod" in phases:
            phase_mod(C, io)
        if cfg.get("dbg_mod"):
            P.add("sp", lambda e: e.dma_start(out=io["modout"], in_=io["modrow"]), reads=["modrow"], writes=["modout"], dma=True)
        if "mix0" in phases:
            phase_inproj0(C, io)
            if cfg.get("overlap", 0):
                import contextlib
                with ExitStack() as st_:
                    ext = {"st": st_, "cm": contextlib.nullcontext()}
                    P.interleave([lambda: phase_lru(C, io, {"st": st_, "cm": contextlib.nullcontext()}),
                                  lambda: phase_mla_prep(C, io, {"st": st_, "cm": contextlib.nullcontext()})], 2)
                P.barrier()
            else:
                phase_lru(C, io)
                phase_mla_prep(C, io)
            phase_mla_attn(C, io)
            phase_outproj(C, io, 0, "w_out_even", io["mixT"], NOWN, list(range(NT_OWN)), {0, 1})
        if "moe0" in phases:
            t0 = cfg.get("moe0_tiles", [list(range(0, 10)), list(range(10, 19))])
            for tl in t0:
                phase_moe(C, io, 0, tl, {0, 1}, io["XL"])
        if "mix1" in phases:
            phase_qkv1(C, io)
            phase_attn1(C, io)
            phase_outproj(C, io, 1, "w_out_odd", io["mixT1"], 2048, list(range(2, 18)), set())
        if "moe1" in phases:
            t1 = cfg.get("moe1_tiles", [list(range(2, 10)), list(range(10, 18))])
            for tl in t1:
                phase_moe(C, io, 1, tl, set(), io["XL"], out_ap=io["out"], out_tiles={t: t - 2 for t in range(2, 18)})
        if cfg.get("dbg_xlout"):
            P.barrier()
            for t in range(NT_OWN):
                P.add("sp", lambda e, t=t: e.dma_start(out=io["XLout"][t * 128:(t + 1) * 128, :], in_=io["XL"][t * 128:(t + 1) * 128, :]),
                      reads=["XL"], writes=["XLout"], dma=True)
        P.barrier()
        P.add("sp", None)
        P.emit()
        cfg["n_ops"] = P.n_ops
    return nc


def local_rows(half):
    lat = np.arange(SEQ) if half == 0 else np.arange(SEQ)[::-1]
    cx = np.arange(NCTX) if half == 0 else np.arange(NCTX)[::-1]
    return lat, cx


def rope_table(pos, rot_dim):
    pos = np.asarray(pos)
    row = (pos // 64).astype(np.float32)
    col = (pos % 64).astype(np.float32)
    n = rot_dim // 4
    freqs = (np.float32(10000.0) ** (-np.arange(n, dtype=np.float32) / np.float32(n))).astype(np.float32)
    ang = np.concatenate([row[:, None] * freqs, col[:, None] * freqs], axis=-1).astype(np.float32)
    return np.ascontiguousarray(np.concatenate([np.cos(ang), np.sin(ang)], axis=-1).astype(np.float32))


def prep_inputs(inputs, used=None):
    f = lambda a: np.ascontiguousarray(np.asarray(a, dtype=np.float32))
    x, c, ctx, c_ctx = f(inputs["x"]), f(inputs["c"]), f(inputs["ctx"]), f(inputs["c_ctx"])
    shared = {}
    shared["w_mod"] = f(inputs["w_mod"])
    shared["b_mod"] = f(inputs["b_mod"])
    nv = np.stack([inputs["norm_mix"][0], inputs["norm_ffn"][0], inputs["norm_mix"][1], inputs["norm_ffn"][1]])
    shared["normT"] = f(np.asarray(nv).reshape(4, 8, 128).transpose(2, 0, 1).reshape(128, 32))
    shared["ident"] = np.eye(128, dtype=np.float32)
    shared["w_router"] = f(inputs["w_router"])
    shared["b_router"] = f(inputs["b_router"])
    wg = np.asarray(inputs["w_gate_up"], dtype=np.float32).reshape(2, NE, 8, 128, 2, 8, 128)
    shared["w_guR"] = np.ascontiguousarray(wg.transpose(0, 1, 5, 3, 4, 2, 6)).reshape(2 * NE * 8, 128, 2048)
    shared["b_guT"] = f(np.asarray(inputs["b_gate_up"]).reshape(2, NE, 16, 128).transpose(0, 3, 1, 2).reshape(2, 128, NE * 16))
    shared["w_down"] = f(inputs["w_down"]).reshape(2 * NE, D, D)
    shared["b_down"] = f(inputs["b_down"]).reshape(2 * NE, D)
    shared["w_in"] = f(inputs["w_in_even"][0])
    shared["q_a_normT"] = f(np.asarray(inputs["mla_q_a_norm"][0]).reshape(2, 128).T)
    shared["kv_a_normT"] = f(np.asarray(inputs["mla_kv_a_norm"][0]).reshape(1, 128).T)
    shared["w_q_b"] = f(inputs["mla_w_q_b"][0])
    shared["w_kv_b"] = f(inputs["mla_w_kv_b"][0])
    shared["nope_norm"] = f(inputs["mla_nope_norm"][0])
    shared["rope_norm"] = f(inputs["mla_rope_norm"][0])
    shared["w_out_even"] = f(inputs["w_out_even"][0])
    shared["w_qkv"] = f(inputs["w_qkv_odd"][0])
    shared["qk_norm"] = f(inputs["gqa_qk_norm"][0])
    shared["sink_rep"] = f(np.repeat(np.asarray(inputs["gqa_sink"][0]).reshape(4, 4, 1), 128, axis=2).reshape(4, 512))
    shared["w_out_odd"] = f(inputs["w_out_odd"][0])
    kk_, qq_ = np.meshgrid(np.arange(128), np.arange(128), indexing="ij")
    shared["masks"] = f(np.stack([(kk_ >= qq_), (kk_ <= qq_)]).astype(np.float32))
    lru = {k: np.asarray(inputs[k][0], dtype=np.float32) for k in
           ("lru_conv_w", "lru_conv_b", "lru_w_r", "lru_b_r", "lru_w_i", "lru_b_i", "lru_lambda")}
    maps = []
    for core in range(8):
        b, half = core // 2, core % 2
        lat, cx = local_rows(half)
        m = dict(shared)
        m["xall"] = f(np.concatenate([ctx[b][cx], x[b][lat]], axis=0))
        vecs = np.stack([c[b], c_ctx])
        m["cT"] = f(vecs.reshape(2, 8, 128).transpose(2, 1, 0).reshape(128, 16))
        lv = np.zeros((128, 2, 4, 8), np.float32)
        wbd = np.zeros((2, 2, 4, 128, 128), np.float32)
        for dl in range(2):
            d = dl if half == 0 else 1 - dl
            for kc in range(4):
                ch = slice(kc * 128, (kc + 1) * 128)
                for j in range(4):
                    lv[:, dl, kc, j] = lru["lru_conv_w"][d, j, ch]
                lv[:, dl, kc, 4] = lru["lru_conv_b"][d, ch]
                lv[:, dl, kc, 5] = lru["lru_b_r"][d, ch]
                lv[:, dl, kc, 6] = lru["lru_b_i"][d, ch]
                lv[:, dl, kc, 7] = lru["lru_lambda"][d, ch]
                for g_, wn in enumerate(("lru_w_r", "lru_w_i")):
                    for bb in range(2):
                        wbd[g_, dl, kc, bb * 64:(bb + 1) * 64, bb * 64:(bb + 1) * 64] = lru[wn][d, kc * 2 + bb]
        m["lruvec"] = f(lv.reshape(128, 64))
        m["wbd"] = f(wbd.reshape(16, 128, 128))
        m["rope_mla"] = rope_table(lat, 32)
        m["rope_gqa"] = rope_table(lat[:2176], 64)
        if used is not None:
            m = {k: v for k, v in m.items() if k in used}
        maps.append(m)
    return maps


def kernel(**inputs):
    cfg = {"phases": ["mod", "mix0", "moe0", "mix1", "moe1"]}
    nc = build_program(cfg)
    maps = prep_inputs(inputs)
    res = run_bass_kernel_spmd(nc, maps, core_ids=list(range(8)))
    out = np.zeros((4, SEQ, D), np.float32)
    for core in range(8):
        b, half = divmod(core, 2)
        lat, _ = local_rows(half)
        out[b, lat[:2048]] = np.asarray(res.results[core]["out"], dtype=np.float32)
    return out
```
